# Optimizing a Trainium2 kernel written in Bass

```python
import jax, jax.numpy as jnp
from jax import lax
import numpy as np

D_MODEL = 1024
BATCH = 1
SEQ = 16384
DEPTH = 2

RET_HEADS = 4
RET_QK_DIM = 128
RET_V_DIM = 256
RET_CHUNK = 128
ROPE_BASE = 10000.0
RET_Q = RET_HEADS * RET_QK_DIM
RET_V = RET_HEADS * RET_V_DIM
LRU_WIDTH = D_MODEL
LRU_BLOCKS = 8
LRU_BLOCK_DIM = LRU_WIDTH // LRU_BLOCKS
CONV_WIDTH = 4
LRU_C = 8.0
MEM_LEN = 256
XATTN_HEADS = 4
XATTN_HEAD_DIM = D_MODEL // XATTN_HEADS
PEER_HEADS = 8
PEER_KEYS = 128
PEER_EXPERTS = PEER_KEYS * PEER_KEYS
PEER_TOPK = 16
PEER_KEY_DIM = 256
PEER_HALF = PEER_KEY_DIM // 2
PEER_TOKEN_BLOCK = 128
EPS = 1e-6
IN_WIDTHS = (RET_Q, RET_Q, RET_V, RET_V, LRU_WIDTH, LRU_WIDTH, D_MODEL, D_MODEL)
IN_COLS = 2 * RET_Q + 2 * RET_V + 2 * LRU_WIDTH + 2 * D_MODEL

kernel_name = "hybrid_retention_rglru_peer_block"


def _rmsnorm(x, g):
    xf = x.astype(jnp.float32)
    y = xf * lax.rsqrt(jnp.mean(xf * xf, axis=-1, keepdims=True) + EPS) * g.astype(jnp.float32)
    return y.astype(x.dtype)


def _split_cols(z, widths):
    outs = []
    off = 0
    for w in widths:
        outs.append(z[..., off:off + w])
        off += w
    return outs


def _rotary(x, positions):
    d = x.shape[-1]
    inv_freq = ROPE_BASE ** (-jnp.arange(0, d, 2, dtype=jnp.float32) / d)
    ang = positions.astype(jnp.float32)[:, :, None] * inv_freq
    cos = jnp.cos(ang)[:, :, None, :]
    sin = jnp.sin(ang)[:, :, None, :]
    x1, x2 = x[..., : d // 2], x[..., d // 2:]
    return jnp.concatenate([x1 * cos - x2 * sin, x2 * cos + x1 * sin], axis=-1)


def _retention(q, k, v, positions):
    B, T, _ = q.shape
    H, dk, dv, C = RET_HEADS, RET_QK_DIM, RET_V_DIM, RET_CHUNK
    n = T // C
    q = _rotary(q.astype(jnp.float32).reshape(B, T, H, dk), positions)
    k = _rotary(k.astype(jnp.float32).reshape(B, T, H, dk), positions) * (dk ** -0.5)
    v = v.astype(jnp.float32).reshape(B, T, H, dv)
    log_g = jnp.log(1.0 - 2.0 ** (-5.0 - jnp.arange(H, dtype=jnp.float32)))
    qc = q.reshape(B, n, C, H, dk)
    kc = k.reshape(B, n, C, H, dk)
    vc = v.reshape(B, n, C, H, dv)
    i = jnp.arange(C, dtype=jnp.float32)
    diff = i[:, None] - i[None, :]
    decay = jnp.where(diff[None] >= 0, jnp.exp(diff[None] * log_g[:, None, None]), 0.0)
    scores = jnp.einsum('bnihd,bnjhd->bnhij', qc, kc) * decay
    inner = jnp.einsum('bnhij,bnjhe->bnihe', scores, vc)
    k_decay = jnp.exp((C - 1.0 - i)[:, None] * log_g[None, :])
    kv = jnp.einsum('bnjhd,bnjhe->nbhde', kc * k_decay[:, :, None], vc)
    chunk_decay = jnp.exp(C * log_g)[None, :, None, None]

    def step(state, kv_c):
        return chunk_decay * state + kv_c, state

    _, s_prev = lax.scan(step, jnp.zeros((B, H, dk, dv), jnp.float32), kv)
    q_decay = jnp.exp((i + 1.0)[:, None] * log_g[None, :])
    cross = jnp.einsum('bnihd,nbhde->bnihe', qc * q_decay[:, :, None], s_prev)
    o = (inner + cross).reshape(B, T, H, dv)
    mu = jnp.mean(o, axis=-1, keepdims=True)
    var = jnp.mean(jnp.square(o - mu), axis=-1, keepdims=True)
    o = (o - mu) * lax.rsqrt(var + EPS)
    return o.reshape(B, T, H * dv)


def _rg_lru_branch(x, positions, conv_w, conv_b, lru_wa, lru_ba, lru_wx, lru_bx, lru_lam):
    B, T, W = x.shape
    xf = x.astype(jnp.float32)
    xc = lax.conv_general_dilated(
        xf, conv_w.astype(jnp.float32)[:, None, :], window_strides=(1,),
        padding=[(CONV_WIDTH - 1, 0)], dimension_numbers=('NWC', 'WIO', 'NWC'),
        feature_group_count=W) + conv_b.astype(jnp.float32)
    xb = xc.reshape(B, T, LRU_BLOCKS, LRU_BLOCK_DIM)
    gate_r = jax.nn.sigmoid(jnp.einsum('btnc,ncd->btnd', xb, lru_wa.astype(jnp.float32))
                            + lru_ba.astype(jnp.float32)).reshape(B, T, W)
    gate_i = jax.nn.sigmoid(jnp.einsum('btnc,ncd->btnd', xb, lru_wx.astype(jnp.float32))
                            + lru_bx.astype(jnp.float32)).reshape(B, T, W)
    log_a = -LRU_C * gate_r * jax.nn.softplus(-lru_lam.astype(jnp.float32))
    a = jnp.exp(log_a)
    mult = jnp.sqrt(-jnp.expm1(2.0 * log_a))
    reset = (positions == 0)[:, :, None]
    a = jnp.where(reset, 0.0, a)
    mult = jnp.where(reset, 1.0, mult)
    b = xc * gate_i * mult

    def combine(c1, c2):
        a1, b1 = c1
        a2, b2 = c2
        return a1 * a2, a2 * b1 + b2

    _, h = lax.associative_scan(combine, (a, b), axis=1)
    return h


def _token_mixer(x, positions, g_mix, w_in, w_ret_br, w_rnn_br, w_mix_out,
                 conv_w, conv_b, lru_wa, lru_ba, lru_wx, lru_bx, lru_lam):
    h = _rmsnorm(x, g_mix)
    z = h @ w_in
    q, k, v, g_ret, x_rnn, y_rnn, gate_a, gate_b = _split_cols(z, IN_WIDTHS)
    ret = (jax.nn.silu(g_ret.astype(jnp.float32)) * _retention(q, k, v, positions)).astype(x.dtype)
    p_ret = ret @ w_ret_br
    rnn = _rg_lru_branch(x_rnn, positions, conv_w, conv_b, lru_wa, lru_ba, lru_wx, lru_bx, lru_lam)
    rnn = (rnn * jax.nn.gelu(y_rnn.astype(jnp.float32))).astype(x.dtype)
    p_rnn = rnn @ w_rnn_br
    merged = jax.nn.sigmoid(gate_a) * p_ret + jax.nn.sigmoid(gate_b) * p_rnn
    return merged @ w_mix_out


def _memory_cross_attention(x, mem, g_x, g_mem, w_xq, w_xk, w_xv, w_xo):
    B, T, D = x.shape
    M = mem.shape[1]
    h = _rmsnorm(x, g_x)
    m = _rmsnorm(mem, g_mem)
    q = (h @ w_xq).reshape(B, T, XATTN_HEADS, XATTN_HEAD_DIM)
    k = (m @ w_xk).reshape(B, M, XATTN_HEADS, XATTN_HEAD_DIM)
    v = (m @ w_xv).reshape(B, M, XATTN_HEADS, XATTN_HEAD_DIM)
    s = jnp.einsum('bthd,bmhd->bhtm', q, k).astype(jnp.float32) * (XATTN_HEAD_DIM ** -0.5)
    p = jax.nn.softmax(s, axis=-1).astype(v.dtype)
    o = jnp.einsum('bhtm,bmhd->bthd', p, v).reshape(B, T, D)
    return o @ w_xo


def _peer(x, g_ffn, w_pq, sub_k1, sub_k2, peer_u, peer_v):
    B, T, D = x.shape
    N = B * T
    K = PEER_TOPK
    hf = _rmsnorm(x, g_ffn).reshape(N, D)
    q = (hf @ w_pq).reshape(N, PEER_HEADS, PEER_KEY_DIM)
    q1, q2 = q[..., :PEER_HALF], q[..., PEER_HALF:]
    s1 = jnp.einsum('thd,hnd->thn', q1, sub_k1).astype(jnp.float32)
    s2 = jnp.einsum('thd,hnd->thn', q2, sub_k2).astype(jnp.float32)
    v1, i1 = lax.top_k(s1, K)
    v2, i2 = lax.top_k(s2, K)
    cand = (v1[..., :, None] + v2[..., None, :]).reshape(N, PEER_HEADS, K * K)
    vals, ci = lax.top_k(cand, K)
    e1 = jnp.take_along_axis(i1, ci // K, axis=-1)
    e2 = jnp.take_along_axis(i2, ci % K, axis=-1)
    idx = (e1 * PEER_KEYS + e2).reshape(N, PEER_HEADS * K)
    gates = jax.nn.softmax(vals, axis=-1).reshape(N, PEER_HEADS * K)
    nb = N // PEER_TOKEN_BLOCK

    def block(args):
        xb, ib, gb = args
        ub = jnp.take(peer_u, ib, axis=0)
        act = jax.nn.gelu(jnp.einsum('tkd,td->tk', ub, xb).astype(jnp.float32))
        vb = jnp.take(peer_v, ib, axis=0)
        return jnp.einsum('tk,tkd->td', (gb * act).astype(vb.dtype), vb)

    out = lax.map(block, (hf.reshape(nb, PEER_TOKEN_BLOCK, D),
                          idx.reshape(nb, PEER_TOKEN_BLOCK, PEER_HEADS * K),
                          gates.reshape(nb, PEER_TOKEN_BLOCK, PEER_HEADS * K)))
    return out.reshape(B, T, D)


def setup_inputs(seed: int = 0) -> dict:
    key = jax.random.key(seed)
    ks = jax.random.split(key, 32)
    f32 = jnp.float32
    L, D = DEPTH, D_MODEL

    def nrm(k, shape, fan_in):
        return jax.random.normal(k, shape, f32) * (fan_in ** -0.5)

    def gain(k, shape):
        return 1.0 + 0.02 * jax.random.normal(k, shape, f32)

    u = jax.random.uniform(ks[13], (L, LRU_WIDTH), f32, 0.9, 0.999)
    s = u ** (1.0 / LRU_C)
    lam = jnp.log(s) - jnp.log1p(-s)
    return {
        "x": jax.random.normal(ks[0], (BATCH, SEQ, D), f32),
        "mem": jax.random.normal(ks[1], (BATCH, MEM_LEN, D), f32),
        "positions": jnp.broadcast_to(jnp.arange(SEQ, dtype=jnp.int32)[None, :], (BATCH, SEQ)),
        "g_mix": gain(ks[2], (L, D)),
        "w_in": nrm(ks[3], (L, D, IN_COLS), D),
        "w_ret_br": nrm(ks[4], (L, RET_V, D), RET_V),
        "w_rnn_br": nrm(ks[5], (L, LRU_WIDTH, D), LRU_WIDTH),
        "w_mix_out": nrm(ks[6], (L, D, D), D),
        "conv_w": nrm(ks[7], (L, CONV_WIDTH, LRU_WIDTH), CONV_WIDTH),
        "conv_b": 0.01 * jax.random.normal(ks[8], (L, LRU_WIDTH), f32),
        "lru_wa": nrm(ks[9], (L, LRU_BLOCKS, LRU_BLOCK_DIM, LRU_BLOCK_DIM), LRU_BLOCK_DIM),
        "lru_ba": 0.01 * jax.random.normal(ks[10], (L, LRU_BLOCKS, LRU_BLOCK_DIM), f32),
        "lru_wx": nrm(ks[11], (L, LRU_BLOCKS, LRU_BLOCK_DIM, LRU_BLOCK_DIM), LRU_BLOCK_DIM),
        "lru_bx": 0.01 * jax.random.normal(ks[12], (L, LRU_BLOCKS, LRU_BLOCK_DIM), f32),
        "lru_lam": lam,
        "g_x": gain(ks[14], (L, D)),
        "g_mem": gain(ks[15], (L, D)),
        "w_xq": nrm(ks[16], (L, D, D), D),
        "w_xk": nrm(ks[17], (L, D, D), D),
        "w_xv": nrm(ks[18], (L, D, D), D),
        "w_xo": nrm(ks[19], (L, D, D), D),
        "g_ffn": gain(ks[20], (L, D)),
        "w_pq": nrm(ks[21], (L, D, PEER_HEADS * PEER_KEY_DIM), D),
        "sub_k1": nrm(ks[22], (L, PEER_HEADS, PEER_KEYS, PEER_HALF), PEER_HALF),
        "sub_k2": nrm(ks[23], (L, PEER_HEADS, PEER_KEYS, PEER_HALF), PEER_HALF),
        "peer_u": nrm(ks[24], (L, PEER_EXPERTS, D), D),
        "peer_v": nrm(ks[25], (L, PEER_EXPERTS, D), D),
        "g_final": gain(ks[26], (D,)),
    }


def reference(x, mem, positions, g_mix, w_in, w_ret_br, w_rnn_br, w_mix_out,
              conv_w, conv_b, lru_wa, lru_ba, lru_wx, lru_bx, lru_lam,
              g_x, g_mem, w_xq, w_xk, w_xv, w_xo,
              g_ffn, w_pq, sub_k1, sub_k2, peer_u, peer_v, g_final):
    for l in range(DEPTH):
        x = x + _token_mixer(x, positions, g_mix[l], w_in[l], w_ret_br[l], w_rnn_br[l], w_mix_out[l],
                             conv_w[l], conv_b[l], lru_wa[l], lru_ba[l], lru_wx[l], lru_bx[l], lru_lam[l])
        x = x + _memory_cross_attention(x, mem, g_x[l], g_mem[l], w_xq[l], w_xk[l], w_xv[l], w_xo[l])
        x = x + _peer(x, g_ffn[l], w_pq[l], sub_k1[l], sub_k2[l], peer_u[l], peer_v[l])
    return _rmsnorm(x, g_final)
```

```python
import math
import numpy as np
import concourse.bass as bass
import concourse.mybir as mybir
from concourse.bass_utils import run_bass_kernel_spmd

F32 = mybir.dt.float32
I32 = mybir.dt.int32
U32 = mybir.dt.uint32
ALU = mybir.AluOpType
AF = mybir.ActivationFunctionType
AX = mybir.AxisListType

D = 1024
NCORES = 8
SEQ = 16384
DEPTH = 2
EPS = 1e-6
ENGS = ("pe", "act", "dve", "pool", "sp")
EPOCH = 30000


class Sched:
    def __init__(self, nc, sems):
        self.nc = nc
        self.free = list(sems)
        self.streams = {e: [] for e in ENGS}
        self.cur = {}
        self.seen = {e: {} for e in ENGS}
        self.lastw = {}
        self.readers = {}
        self.latest = {}
        nd = {"sp": 24, "pool": 20}
        self.dma_sems = {q: [self.free.pop() for _ in range(n)] for q, n in nd.items()}
        self.dma_rr = {"sp": 0, "pool": 0}
        self.dma_use = {}
        self.nops = 0

    def _tok(self, eng):
        c = self.cur.get(eng)
        if c is None or c[1] >= EPOCH:
            c = [self.free.pop(), 0]
            self.cur[eng] = c
        c[1] += 1
        return (c[0], c[1])

    def op(self, eng, fn, reads=(), writes=(), dma=False):
        waits = {}
        seen = self.seen[eng]

        def need(tok):
            if tok is None:
                return
            s, v = tok
            if seen.get(s, 0) >= v:
                return
            if waits.get(s, 0) < v:
                waits[s] = v

        for k in reads:
            need(self.lastw.get(k))
        for k in writes:
            need(self.lastw.get(k))
            for s, v in self.readers.get(k, {}).items():
                need((s, v))
        if dma:
            sl = self.dma_sems[eng]
            s = sl[self.dma_rr[eng] % len(sl)]
            self.dma_rr[eng] += 1
            prev = self.dma_use.get(s, 0)
            need((s, prev))
            tok = (s, prev + 16)
            self.dma_use[s] = prev + 16
            inc = (s, 16)
        else:
            tok = self._tok(eng)
            inc = (tok[0], 1)
            if eng == "pe":
                seen[tok[0]] = tok[1]
        for s, v in waits.items():
            seen[s] = v
        self.latest[tok[0]] = tok[1]
        for k in reads:
            r = self.readers.setdefault(k, {})
            if r.get(tok[0], 0) < tok[1]:
                r[tok[0]] = tok[1]
        for k in writes:
            self.lastw[k] = tok
            self.readers[k] = {}
        self.streams[eng].append((list(waits.items()), fn, inc))
        self.nops += 1
        return tok

    def barrier(self):
        for e in ENGS:
            waits = []
            for s, v in self.latest.items():
                if self.seen[e].get(s, 0) < v:
                    waits.append((s, v))
                    self.seen[e][s] = v
            if waits:
                self.streams[e].append((waits, None, None))
        self.lastw = {}
        self.readers = {}

    def emit(self):
        nc = self.nc

        def run(e, name):
            for waits, fn, inc in self.streams[name]:
                for s, v in waits:
                    e.wait_ge(s, v)
                if fn is not None:
                    ins = fn(e)
                    ins.then_inc(inc[0], inc[1])

        with nc.Block() as block:
            @block.sync
            def _(e):
                run(e, "sp")

            @block.tensor
            def _(e):
                run(e, "pe")

            @block.vector
            def _(e):
                run(e, "dve")

            @block.scalar
            def _(e):
                run(e, "act")

            @block.gpsimd
            def _(e):
                run(e, "pool")


class Arena:
    def __init__(self, nc, base, limit):
        self.nc, self.base, self.limit = nc, base, limit
        self.ptr = base
        self.n = 0

    def reset(self):
        self.ptr = self.base

    def keep(self):
        self.base = self.ptr

    def t(self, shape, dtype=F32):
        nbytes = 4
        for s in shape[1:]:
            nbytes *= s
        self.n += 1
        off = (self.ptr + 63) // 64 * 64
        assert off + nbytes <= self.limit, (off, nbytes, self.limit)
        h = self.nc.alloc_sbuf_tensor_at(f"sb{self.n}", list(shape), dtype, offset=off)
        self.ptr = off + nbytes
        return h


class K:
    def __init__(self, T, phase, last):
        self.T, self.phase, self.last = T, phase, last
        self.NT = T // 128
        self.BLK = min(512, T)
        nc = bass.Bass("TRN2", target_bir_lowering=False)
        self.nc = nc
        self.stack = []
        sems = []
        import contextlib
        self.es = contextlib.ExitStack()
        for i in range(80):
            sems.append(self.es.enter_context(nc.semaphore(f"s{i}")))
        self.S = Sched(nc, sems)
        self.ar = Arena(nc, 16384, 16384 + 212000)
        self.ps = [nc.alloc_psum_tensor(f"ps{i}", [128, 512], F32) for i in range(8)]
        self.psk = [("ps", i) for i in range(8)]
        self.uid = 0
        self.regc = {}
        self.din = {}
        self.dout = {}

    def inp(self, name, shape, dtype=F32):
        a = self.nc.dram_tensor(name, list(shape), dtype, kind="ExternalInput").ap()
        self.din[name] = a
        return a

    def outp(self, name, shape, dtype=F32):
        a = self.nc.dram_tensor(name, list(shape), dtype, kind="ExternalOutput").ap()
        self.dout[name] = a
        return a

    def scratch(self, name, shape, dtype=F32):
        return self.nc.dram_tensor(name, list(shape), dtype).ap()

    def key(self, base="b"):
        self.uid += 1
        return (base, self.uid)

    def dma(self, out, in_, reads=(), writes=(), q="sp", slow=False):
        if slow:
            return self.S.op(q, lambda e: e.dma_start(out=out, in_=in_, allow_slow_non_contiguous=True),
                             reads, writes, dma=True)
        return self.S.op(q, lambda e: e.dma_start(out=out, in_=in_), reads, writes, dma=True)

    def mm(self, out, lhsT, rhs, start, stop, reads, writes):
        return self.S.op("pe", lambda e: e.matmul(out, lhsT, rhs, start=start, stop=stop), reads, writes)

    def tr(self, out, in_, ident, reads, writes):
        return self.S.op("pe", lambda e: e.transpose(out, in_, ident), reads, writes)

    def act(self, out, in_, func, reads, writes, bias=0.0, scale=1.0, accum_out=None):
        if accum_out is None:
            f = lambda e: e.activation(out, in_, func, bias=bias, scale=scale)
        else:
            f = lambda e: e.activation(out, in_, func, bias=bias, scale=scale, accum_out=accum_out)
        return self.S.op("act", f, reads, writes)

    def dve(self, f, reads, writes):
        return self.S.op("dve", f, reads, writes)

    def pool(self, f, reads, writes):
        return self.S.op("pool", f, reads, writes)

    def consts(self):
        ar = self.ar
        self.ident = ar.t([128, 128])
        kI = self.kI = ("ident",)
        ones = ar.t([128, 128])
        kO = self.key()
        self.pool(lambda e: e.memset(ones[:], 1.0), [], [kO])
        self.pool(lambda e: e.affine_select(self.ident[:], ones[:], pattern=[[-1, 128]],
                                            compare_op=ALU.is_equal, fill=0.0, base=0,
                                            channel_multiplier=1), [kO], [kI])
        self.ones = ones
        self.kOnes = kO
        ar.keep()

    def rmsnorm_rows(self, xt, kx, gb, kg, h, kh, ss, kss, junk, kj, rows=128):
        self.act(junk[0:rows, :], xt[0:rows, :], AF.Square, [kx], [kj, kss], accum_out=ss[0:rows, 0:1])
        self.dve(lambda e: e.tensor_scalar(ss[0:rows, 1:2], ss[0:rows, 0:1], 1.0 / D, EPS, ALU.mult, ALU.add),
                 [kss], [kss])
        self.act(ss[0:rows, 3:4], ss[0:rows, 1:2], AF.Sqrt, [kss], [kss])
        self.dve(lambda e: e.reciprocal(ss[0:rows, 2:3], ss[0:rows, 3:4]), [kss], [kss])
        self.dve(lambda e: e.scalar_tensor_tensor(h[0:rows, :], xt[0:rows, :], ss[0:rows, 2:3], gb[0:rows, :],
                                                  ALU.mult, ALU.mult), [kx, kss, kg], [kh])

    def load_bcast(self, dst, dram_vec, kd):
        self.dma(dst, dram_vec.partition_broadcast(128), [], [kd])

    def stage_A(self, x_d, xh_d, w_in, g_mix, pos_d, invf_d, ztm, zfm, zh):
        T, NT, BLK = self.T, self.NT, self.BLK
        ar = self.ar
        ar.reset()
        S = self.S
        gb = ar.t([128, D]); kg = self.key()
        self.load_bcast(gb[:], g_mix, kg)
        NF = 64
        posi = ar.t([128, NT], I32); kp = self.key()
        self.dma(posi[:], pos_d.rearrange("(n p) -> p n", p=128), [], [kp], slow=True)
        posf = ar.t([128, NT]); kpf = self.key()
        self.dve(lambda e: e.tensor_copy(posf[:], posi[:]), [kp], [kpf])
        invf = ar.t([128, NF]); kiv = self.key()
        self.load_bcast(invf[:], invf_d, kiv)
        ang = ar.t([128, NT, NF]); ka = self.key()
        self.dve(lambda e: e.tensor_tensor(ang[:], posf[:].unsqueeze(2).broadcast_to([128, NT, NF]),
                                           invf[:].unsqueeze(1).broadcast_to([128, NT, NF]), ALU.mult),
                 [kpf, kiv], [ka])
        kq = ar.t([128, NT, NF]); kkq = self.key()
        kqi = ar.t([128, NT, NF], I32); kkqi = self.key()
        TWO_PI = 2.0 * math.pi
        self.dve(lambda e: e.tensor_scalar(kq[:], ang[:], 1.0 / TWO_PI, None, ALU.mult), [ka], [kkq])
        self.dve(lambda e: e.tensor_copy(kqi[:], kq[:]), [kkq], [kkqi])
        self.dve(lambda e: e.tensor_copy(kq[:], kqi[:]), [kkqi], [kkq])
        C1 = 6.28125
        c2 = TWO_PI - C1
        C2 = float(np.float32(np.round(c2 * 2 ** 20) / 2 ** 20))
        C3 = float(np.float32(c2 - C2))
        r = ar.t([128, NT, NF]); kr = self.key()
        self.dve(lambda e: e.scalar_tensor_tensor(r[:], kq[:], -C1, ang[:], ALU.mult, ALU.add), [kkq, ka], [kr])
        self.dve(lambda e: e.scalar_tensor_tensor(r[:], kq[:], -C2, r[:], ALU.mult, ALU.add), [kkq, kr], [kr])
        self.dve(lambda e: e.scalar_tensor_tensor(r[:], kq[:], -C3, r[:], ALU.mult, ALU.add), [kkq, kr], [kr])
        tmp = ar.t([128, NT, NF]); kt = self.key()

        def wrap(buf, kb):
            self.dve(lambda e: e.tensor_scalar(tmp[:], buf[:], math.pi, -TWO_PI, ALU.is_gt, ALU.mult), [kb], [kt])
            self.dve(lambda e: e.tensor_tensor(buf[:], buf[:], tmp[:], ALU.add), [kb, kt], [kb])
            self.dve(lambda e: e.tensor_scalar(tmp[:], buf[:], -math.pi, TWO_PI, ALU.is_lt, ALU.mult), [kb], [kt])
            self.dve(lambda e: e.tensor_tensor(buf[:], buf[:], tmp[:], ALU.add), [kb, kt], [kb])
            self.dve(lambda e: e.tensor_scalar(buf[:], buf[:], math.pi, -math.pi, ALU.min, ALU.max), [kb], [kb])

        wrap(r, kr)
        r2 = ar.t([128, NT, NF]); kr2 = self.key()
        self.dve(lambda e: e.tensor_scalar(r2[:], r[:], math.pi / 2, None, ALU.add), [kr], [kr2])
        wrap(r2, kr2)
        sin_t = ar.t([128, NT, NF]); cos_t = ar.t([128, NT, NF]); ksc = self.key()
        self.act(sin_t[:], r[:], AF.Sin, [kr], [ksc])
        self.act(cos_t[:], r2[:], AF.Sin, [kr2], [ksc])
        sink_t = ar.t([128, NT, NF]); cosk_t = ar.t([128, NT, NF]); ksck = self.key()
        sc = 128 ** -0.5
        self.dve(lambda e: e.tensor_scalar(sink_t[:], sin_t[:], sc, None, ALU.mult), [ksc], [ksck])
        self.dve(lambda e: e.tensor_scalar(cosk_t[:], cos_t[:], sc, None, ALU.mult), [ksc], [ksck])

        NB = BLK // 128
        hT = ar.t([128, 8, BLK]); khT = [self.key() for _ in range(NB)]
        xt = [ar.t([128, D]) for _ in range(2)]; kx = [self.key() for _ in range(2)]
        hb = [ar.t([128, D]) for _ in range(2)]; kh = [self.key() for _ in range(2)]
        junk = ar.t([128, D]); kj = self.key()
        ss = [ar.t([128, 4]) for _ in range(2)]; kss = [self.key() for _ in range(2)]
        W = [ar.t([128, 8, 512]) for _ in range(2)]; kW = [self.key() for _ in range(2)]
        st = [ar.t([128, 512]) for _ in range(4)]; kst = [self.key() for _ in range(4)]
        rt = [ar.t([128, 4, 4, 64]) for _ in range(2)]; krt = [self.key() for _ in range(2)]
        hhT = ar.t([128, 8, 4]); khh = self.key()
        sth = ar.t([128, 8, 4]); ksth = self.key()
        w_in_v = w_in.rearrange("(kc p) c -> p kc c", p=128)
        x_v = x_d.rearrange("(n p) c -> n p c", p=128)
        ztm_v = ztm.rearrange("(n p) c -> n p c", p=128)
        psi = 0
        wi = 0
        sti = 0
        ti = 0

        def norm_T(src_ap, rows, dstT_fn, kdst):
            nonlocal ti, psi
            b = ti % 2
            ti += 1
            self.dma(xt[b][0:rows, :], src_ap, [], [kx[b]])
            self.rmsnorm_rows(xt[b], kx[b], gb, kg, hb[b], kh[b], ss[b], kss[b], junk, kj, rows=rows)
            for half in range(2):
                p = psi % 8
                psi += 1
                for j in range(4):
                    kc = half * 4 + j
                    self.tr(self.ps[p][:, j * 128:j * 128 + rows], hb[b][0:rows, kc * 128:(kc + 1) * 128],
                            self.ident[0:rows, 0:rows], [kh[b], self.kI], [self.psk[p]])
                dstT_fn(half, p)

        if xh_d is not None:
            def dst_h(half, p):
                self.act(hhT[:, half * 4:(half + 1) * 4, 0:3],
                         self.ps[p][:].rearrange("p (j c) -> p j c", c=128)[:, :, 0:3], AF.Copy,
                         [self.psk[p]], [khh])
            norm_T(xh_d, 3, dst_h, khh)

        for blk in range(T // BLK):
            for i in range(NB):
                tile = blk * NB + i

                def dst_t(half, p, i=i):
                    self.act(hT[:, half * 4:(half + 1) * 4, i * 128:(i + 1) * 128],
                             self.ps[p][:].rearrange("p (j c) -> p j c", c=128), AF.Copy,
                             [self.psk[p]], [khT[i]])
                norm_T(x_v[tile], 128, dst_t, khT[i])
            for grp in range(14):
                wb = wi % 2
                wi += 1
                self.dma(W[wb][:], w_in_v[:, :, grp * 512:(grp + 1) * 512], [], [kW[wb]])
                if grp < 6:
                    for i in range(NB):
                        tile = blk * NB + i
                        p = psi % 8
                        psi += 1
                        for kc in range(8):
                            self.mm(self.ps[p][:], hT[:, kc, i * 128:(i + 1) * 128], W[wb][:, kc, :],
                                    kc == 0, kc == 7, [khT[i], kW[wb]], [self.psk[p]])
                        sb = sti % 4
                        sti += 1
                        if grp < 2:
                            ct, sn = (cos_t, sin_t) if grp == 0 else (cosk_t, sink_t)
                            kt_ = ksc if grp == 0 else ksck
                            pv = self.ps[p][:].rearrange("p (h two f) -> p h two f", two=2, f=64)
                            x1 = pv[:, :, 0, :]
                            x2 = pv[:, :, 1, :]
                            cb = ct[:, tile, :].unsqueeze(1).broadcast_to([128, 4, 64])
                            sb_ = sn[:, tile, :].unsqueeze(1).broadcast_to([128, 4, 64])
                            ov = st[sb][:].rearrange("p (h two f) -> p h two f", two=2, f=64)
                            rb = (sti) % 2
                            R = rt[rb]
                            self.dve(lambda e, R=R, x1=x1, cb=cb: e.tensor_tensor(R[:, 0], x1, cb, ALU.mult),
                                     [self.psk[p], kt_], [krt[rb]])
                            self.dve(lambda e, R=R, x2=x2, sb_=sb_: e.tensor_tensor(R[:, 1], x2, sb_, ALU.mult),
                                     [self.psk[p], kt_], [krt[rb]])
                            self.dve(lambda e, R=R, x2=x2, cb=cb: e.tensor_tensor(R[:, 2], x2, cb, ALU.mult),
                                     [self.psk[p], kt_], [krt[rb]])
                            self.dve(lambda e, R=R, x1=x1, sb_=sb_: e.tensor_tensor(R[:, 3], x1, sb_, ALU.mult),
                                     [self.psk[p], kt_], [krt[rb]])
                            self.dve(lambda e, R=R, ov=ov: e.tensor_tensor(ov[:, :, 0, :], R[:, 0], R[:, 1], ALU.subtract),
                                     [krt[rb]], [kst[sb]])
                            self.dve(lambda e, R=R, ov=ov: e.tensor_tensor(ov[:, :, 1, :], R[:, 2], R[:, 3], ALU.add),
                                     [krt[rb]], [kst[sb]])
                        else:
                            self.act(st[sb][:], self.ps[p][:], AF.Copy, [self.psk[p]], [kst[sb]])
                        self.dma(ztm_v[tile][:, grp * 512:(grp + 1) * 512], st[sb][:], [kst[sb]], [])
                else:
                    for sub in range(4):
                        c = (grp - 6) * 4 + sub
                        p = psi % 8
                        psi += 1
                        for kc in range(8):
                            self.mm(self.ps[p][:, 0:BLK], W[wb][:, kc, sub * 128:(sub + 1) * 128], hT[:, kc, :],
                                    kc == 0, kc == 7, khT + [kW[wb]], [self.psk[p]])
                        sb = sti % 4
                        sti += 1
                        self.act(st[sb][:, 0:BLK], self.ps[p][:, 0:BLK], AF.Copy, [self.psk[p]], [kst[sb]])
                        self.dma(zfm[c][:, blk * BLK:(blk + 1) * BLK], st[sb][:, 0:BLK], [kst[sb]], [])
                        if xh_d is not None and blk == 0 and c < 8:
                            p = psi % 8
                            psi += 1
                            for kc in range(8):
                                self.mm(self.ps[p][:, 0:3], W[wb][:, kc, sub * 128:(sub + 1) * 128], hhT[:, kc, 0:3],
                                        kc == 0, kc == 7, [khh, kW[wb]], [self.psk[p]])
                            self.act(sth[:, c, 0:3], self.ps[p][:, 0:3], AF.Copy, [self.psk[p]], [ksth])
            if xh_d is not None and blk == 0:
                self.dma(zh.rearrange("c p t -> p c t"), sth[:, :, 0:3], [ksth], [], slow=True)
        S.barrier()


def build_test_A(T):
    k = K(T, "A", False)
    x = k.inp("x", [T, D]); xh = k.inp("xh", [3, D]); w_in = k.inp("w_in", [D, 7168])
    g = k.inp("g_mix", [D]); pos = k.inp("pos", [T], I32); invf = k.inp("invf", [64])
    ztm = k.outp("ztm", [T, 3072]); zfm = k.outp("zfm", [32, 128, T]); zh = k.outp("zh", [8, 128, 3])
    k.consts()
    k.stage_A(x, xh, w_in, g, pos, invf, ztm, zfm, zh)
    k.S.emit()
    return k


LOG_G = [math.log(1.0 - 2.0 ** (-5.0 - h)) for h in range(4)]


def _ret_tables(self):
    ar = self.ar
    kdec = ar.t([128, 4]); qdT = ar.t([128, 4, 128]); decT = ar.t([128, 4, 128])
    tmpc = ar.t([128, 1]); tmpm = ar.t([128, 128]); tmpr = ar.t([128, 128])
    kt = self.key()
    self.kret = kt
    self.pool(lambda e: e.iota(tmpc[:], pattern=[[0, 1]], base=127, channel_multiplier=-1, allow_small_or_imprecise_dtypes=True), [], [kt])
    self.pool(lambda e: e.iota(tmpm[:], pattern=[[1, 128]], base=0, channel_multiplier=-1, allow_small_or_imprecise_dtypes=True), [], [kt])
    self.pool(lambda e: e.iota(tmpr[:], pattern=[[1, 128]], base=1, channel_multiplier=0, allow_small_or_imprecise_dtypes=True), [], [kt])
    for h in range(4):
        self.act(kdec[:, h:h + 1], tmpc[:], AF.Exp, [kt], [kt], scale=LOG_G[h])
        self.act(qdT[:, h, :], tmpr[:], AF.Exp, [kt], [kt], scale=LOG_G[h])
        self.act(decT[:, h, :], tmpm[:], AF.Exp, [kt], [kt], scale=LOG_G[h])
        self.pool(lambda e, h=h: e.affine_select(decT[:, h, :], decT[:, h, :], pattern=[[1, 128]],
                                                 compare_op=ALU.is_ge, fill=0.0, base=0,
                                                 channel_multiplier=-1), [kt], [kt])
    self.kdec, self.qdT, self.decT = kdec, qdT, decT


def stage_B1(self, ztm, send_out):
    T, NT = self.T, self.NT
    ar = self.ar
    ar.reset()
    _ret_tables(self)
    kt = self.kret
    ztm_v = ztm.rearrange("(n p) c -> n p c", p=128)
    kb = [ar.t([128, 512]) for _ in range(2)]; kkb = [self.key() for _ in range(2)]
    vb = [ar.t([128, 1024]) for _ in range(2)]; kvb = [self.key() for _ in range(2)]
    kd = [ar.t([128, 4, 128]) for _ in range(2)]; kkd = [self.key() for _ in range(2)]
    Sb = [ar.t([128, 4, 256]) for _ in range(2)]; kS = [self.key() for _ in range(2)]
    self.dve(lambda e: e.memset(Sb[0][:], 0.0), [], [kS[0]])
    psi = 0
    for n in range(NT):
        b = n % 2
        self.dma(kb[b][:], ztm_v[n][:, 512:1024], [], [kkb[b]])
        self.dma(vb[b][:], ztm_v[n][:, 1024:2048], [], [kvb[b]])
        self.dve(lambda e, b=b: e.tensor_tensor(kd[b][:], kb[b][:].rearrange("p (h d) -> p h d", h=4),
                                                self.kdec[:].unsqueeze(2).broadcast_to([128, 4, 128]), ALU.mult),
                 [kkb[b], kt], [kkd[b]])
        so, sn = Sb[n % 2], Sb[(n + 1) % 2]
        for hp in range(2):
            p = psi % 8
            psi += 1
            for e2 in range(2):
                h = hp * 2 + e2
                self.mm(self.ps[p][:, e2 * 256:(e2 + 1) * 256], kd[b][:, h, :], vb[b][:, h * 256:(h + 1) * 256],
                        True, True, [kkd[b], kvb[b]], [self.psk[p]])
            for e2 in range(2):
                h = hp * 2 + e2
                cd = math.exp(128 * LOG_G[h])
                self.dve(lambda e, h=h, cd=cd, p=p, e2=e2, so=so, sn=sn: e.scalar_tensor_tensor(
                    sn[:, h, :], so[:, h, :], cd, self.ps[p][:, e2 * 256:(e2 + 1) * 256], ALU.mult, ALU.add),
                    [kS[n % 2], self.psk[p]], [kS[(n + 1) % 2]])
    self.dma(send_out, Sb[NT % 2][:].rearrange("p h e -> p (h e)"), [kS[NT % 2]], [])
    self.S.barrier()


def stage_B2(self, ztm, send_all, csel_d, retT_d):
    T, NT = self.T, self.NT
    ar = self.ar
    ar.reset()
    _ret_tables(self)
    kt = self.kret
    ztm_v = ztm.rearrange("(n p) c -> n p c", p=128)
    csel = ar.t([128, 8]); kcs = self.key()
    self.load_bcast(csel[:], csel_d, kcs)
    fD = ar.t([128, 8, 4]); kfD = self.key()
    for h in range(4):
        Dh = math.exp(T * LOG_G[h])
        self.dve(lambda e, h=h, Dh=Dh: e.tensor_scalar(fD[:, :, h], csel[:], Dh - 1.0, 1.0, ALU.mult, ALU.add),
                 [kcs], [kfD])
    Sb = [ar.t([128, 4, 256]) for _ in range(2)]; kS = [self.key() for _ in range(2)]
    se = [ar.t([128, 1024]) for _ in range(2)]; kse = [self.key() for _ in range(2)]
    self.dve(lambda e: e.memset(Sb[0][:], 0.0), [], [kS[0]])
    for c in range(NCORES):
        b = c % 2
        self.dma(se[b][:], send_all[c], [], [kse[b]])
        self.dve(lambda e, b=b, c=c: e.tensor_scalar(se[b][:], se[b][:], csel[:, c:c + 1], None, ALU.mult),
                 [kse[b], kcs], [kse[b]])
        for h in range(4):
            self.dve(lambda e, b=b, c=c, h=h: e.scalar_tensor_tensor(
                Sb[0][:, h, :], Sb[0][:, h, :], fD[:, c, h:h + 1], se[b][:, h * 256:(h + 1) * 256],
                ALU.mult, ALU.add), [kS[0], kfD, kse[b]], [kS[0]])
    qk = [ar.t([128, 1024]) for _ in range(2)]; kqk = [self.key() for _ in range(2)]
    vb = [ar.t([128, 1024]) for _ in range(2)]; kvb = [self.key() for _ in range(2)]
    gb = [ar.t([128, 1024]) for _ in range(2)]; kgb = [self.key() for _ in range(2)]
    qT = ar.t([128, 4, 128]); kqT = self.key()
    qdT = ar.t([128, 4, 128]); kqd = self.key()
    kT = ar.t([128, 4, 128]); kkT = self.key()
    kd = ar.t([128, 4, 128]); kkd = self.key()
    sT = ar.t([128, 4, 128]); ksT = self.key()
    s12 = ar.t([128, 8]); mv2 = ar.t([128, 8]); rs = ar.t([128, 4]); sq = ar.t([128, 4]); kst = self.key()
    on = ar.t([128, 1024]); kon = self.key()
    sg = ar.t([128, 1024]); ksg = self.key()
    rT = [ar.t([128, 8, 128]) for _ in range(2)]; krT = [self.key() for _ in range(2)]
    retT_v = retT_d.rearrange("c p t -> p c t")
    psi = 0

    def nb():
        nonlocal psi
        p = psi % 8
        psi += 1
        return p

    for n in range(NT):
        b = n % 2
        self.dma(qk[b][:], ztm_v[n][:, 0:1024], [], [kqk[b]])
        self.dma(vb[b][:], ztm_v[n][:, 1024:2048], [], [kvb[b]])
        self.dma(gb[b][:], ztm_v[n][:, 2048:3072], [], [kgb[b]])
        pq, pk = nb(), nb()
        for h in range(4):
            self.tr(self.ps[pq][:, h * 128:(h + 1) * 128], qk[b][:, h * 128:(h + 1) * 128], self.ident[:],
                    [kqk[b], self.kI], [self.psk[pq]])
        for h in range(4):
            self.tr(self.ps[pk][:, h * 128:(h + 1) * 128], qk[b][:, 512 + h * 128:512 + (h + 1) * 128], self.ident[:],
                    [kqk[b], self.kI], [self.psk[pk]])
        self.act(qT[:].rearrange("p h i -> p (h i)"), self.ps[pq][:], AF.Copy, [self.psk[pq]], [kqT])
        self.dve(lambda e: e.tensor_tensor(qdT[:].rearrange("p h i -> p (h i)"), qT[:].rearrange("p h i -> p (h i)"),
                                           self.qdT[:].rearrange("p h i -> p (h i)"), ALU.mult),
                 [kqT, kt], [kqd])
        self.act(kT[:].rearrange("p h i -> p (h i)"), self.ps[pk][:], AF.Copy, [self.psk[pk]], [kkT])
        self.dve(lambda e, b=b: e.tensor_tensor(kd[:], qk[b][:, 512:1024].rearrange("p (h d) -> p h d", h=4),
                                                self.kdec[:].unsqueeze(2).broadcast_to([128, 4, 128]), ALU.mult),
                 [kqk[b], kt], [kkd])
        pc = nb()
        for h in range(4):
            self.mm(self.ps[pc][:, h * 128:(h + 1) * 128], kT[:, h, :], qT[:, h, :], True, True,
                    [kkT, kqT], [self.psk[pc]])
        self.dve(lambda e, pc=pc: e.tensor_tensor(sT[:].rearrange("p h i -> p (h i)"), self.ps[pc][:],
                                                  self.decT[:].rearrange("p h i -> p (h i)"), ALU.mult),
                 [self.psk[pc], kt], [ksT])
        so, sn = Sb[n % 2], Sb[(n + 1) % 2]
        po = [nb(), nb()]
        for h in range(4):
            p = po[h // 2]
            o_ap = self.ps[p][:, (h % 2) * 256:(h % 2 + 1) * 256]
            self.mm(o_ap, sT[:, h, :], vb[b][:, h * 256:(h + 1) * 256], True, False, [ksT, kvb[b]], [self.psk[p]])
            self.mm(o_ap, qdT[:, h, :], so[:, h, :], False, True, [kqd, kS[n % 2]], [self.psk[p]])
        pkv = [nb(), nb()]
        for h in range(4):
            p = pkv[h // 2]
            self.mm(self.ps[p][:, (h % 2) * 256:(h % 2 + 1) * 256], kd[:, h, :], vb[b][:, h * 256:(h + 1) * 256],
                    True, True, [kkd, kvb[b]], [self.psk[p]])
        for h in range(4):
            p = pkv[h // 2]
            cd = math.exp(128 * LOG_G[h])
            self.dve(lambda e, h=h, cd=cd, p=p, so=so, sn=sn: e.scalar_tensor_tensor(
                sn[:, h, :], so[:, h, :], cd, self.ps[p][:, (h % 2) * 256:(h % 2 + 1) * 256], ALU.mult, ALU.add),
                [kS[n % 2], self.psk[p]], [kS[(n + 1) % 2]])
        for h in range(4):
            p = po[h // 2]
            o_ap = self.ps[p][:, (h % 2) * 256:(h % 2 + 1) * 256]
            self.act(sg[:, 0:256], o_ap, AF.Copy, [self.psk[p]], [ksg, kst], accum_out=s12[:, h:h + 1])
            self.act(sg[:, 256:512], o_ap, AF.Square, [self.psk[p]], [ksg, kst], accum_out=s12[:, 4 + h:5 + h])
        self.dve(lambda e: e.tensor_scalar(mv2[:], s12[:], 1.0 / 256.0, None, ALU.mult), [kst], [kst])
        self.dve(lambda e: e.tensor_tensor(sq[:], mv2[:, 0:4], mv2[:, 0:4], ALU.mult), [kst], [kst])
        self.dve(lambda e: e.tensor_tensor(sq[:], mv2[:, 4:8], sq[:], ALU.subtract), [kst], [kst])
        self.dve(lambda e: e.tensor_scalar(sq[:], sq[:], EPS, None, ALU.add), [kst], [kst])
        self.act(sq[:], sq[:], AF.Sqrt, [kst], [kst])
        self.dve(lambda e: e.reciprocal(rs[:], sq[:]), [kst], [kst])
        for h in range(4):
            p = po[h // 2]
            o_ap = self.ps[p][:, (h % 2) * 256:(h % 2 + 1) * 256]
            self.dve(lambda e, h=h, o_ap=o_ap: e.tensor_scalar(on[:, h * 256:(h + 1) * 256], o_ap, mv2[:, h:h + 1],
                                                               rs[:, h:h + 1], ALU.subtract, ALU.mult),
                     [self.psk[p], kst], [kon])
        self.act(sg[:], gb[b][:], AF.Sigmoid, [kgb[b], kst], [ksg])
        self.dve(lambda e, b=b: e.tensor_tensor(sg[:], sg[:], gb[b][:], ALU.mult), [ksg, kgb[b]], [ksg])
        self.dve(lambda e: e.tensor_tensor(on[:], on[:], sg[:], ALU.mult), [kon, ksg], [kon])
        for half in range(2):
            p = nb()
            for j in range(4):
                kc = half * 4 + j
                self.tr(self.ps[p][:, j * 128:(j + 1) * 128], on[:, kc * 128:(kc + 1) * 128], self.ident[:],
                        [kon, self.kI], [self.psk[p]])
            self.act(rT[b][:, half * 4:(half + 1) * 4, :], self.ps[p][:].rearrange("p (j c) -> p j c", c=128),
                     AF.Copy, [self.psk[p]], [krT[b]])
        self.dma(retT_v[:, :, n * 128:(n + 1) * 128], rT[b][:], [krT[b]], [])
    self.S.barrier()


def gelu_tanh(self, out, y, t1, reads, kout, kt1):
    self.dve(lambda e: e.tensor_tensor(t1, y, y, ALU.mult), reads, [kt1])
    self.dve(lambda e: e.tensor_scalar(t1, t1, 0.044715, 1.0, ALU.mult, ALU.add), [kt1], [kt1])
    self.dve(lambda e: e.tensor_tensor(t1, t1, y, ALU.mult), [kt1] + list(reads), [kt1])
    self.act(t1, t1, AF.Sigmoid, [kt1], [kt1], scale=1.5957691216057308)
    self.dve(lambda e: e.tensor_tensor(out, y, t1, ALU.mult), [kt1] + list(reads), [kout])


def stage_C(self, zfm, zh, pos_d, conv_w, conv_b, wa_d, ba_d, wx_d, bx_d, lam_d, mode, lsum_out=None,
            lsum_all=None, csel_d=None, rnnT_d=None):
    T = self.T
    ar = self.ar
    ar.reset()
    kc_ = self.key()
    cw = ar.t([128, 4, 8]); cb_ = ar.t([128, 8]); ba = ar.t([128, 8]); bx = ar.t([128, 8]); lam = ar.t([128, 8])
    for j in range(4):
        self.dma(cw[:, j, :], conv_w[j].rearrange("(cb p) -> p cb", p=128), [], [kc_], slow=True)
    self.dma(cb_[:], conv_b.rearrange("(cb p) -> p cb", p=128), [], [kc_], slow=True)
    self.dma(ba[:], ba_d.rearrange("cb p -> p cb"), [], [kc_], slow=True)
    self.dma(bx[:], bx_d.rearrange("cb p -> p cb"), [], [kc_], slow=True)
    self.dma(lam[:], lam_d.rearrange("(cb p) -> p cb", p=128), [], [kc_], slow=True)
    wa = ar.t([128, 8, 128]); wx = ar.t([128, 8, 128]); kw = self.key()
    self.dma(wa[:], wa_d.rearrange("cb c d -> c cb d"), [], [kw])
    self.dma(wx[:], wx_d.rearrange("cb c d -> c cb d"), [], [kw])
    nsp = ar.t([128, 8]); knsp = self.key()
    self.act(nsp[:], lam[:], AF.Exp, [kc_], [knsp], scale=-1.0)
    self.dve(lambda e: e.tensor_scalar(nsp[:], nsp[:], 1.0, None, ALU.add), [knsp], [knsp])
    self.act(nsp[:], nsp[:], AF.Ln, [knsp], [knsp])
    self.dve(lambda e: e.tensor_scalar(nsp[:], nsp[:], -8.0, None, ALU.mult), [knsp], [knsp])
    posb = ar.t([128, T], I32); kpb = self.key()
    self.dma(posb[:], pos_d.partition_broadcast(128), [], [kpb])
    notm = ar.t([128, T]); m0 = ar.t([128, T]); km = self.key()
    self.dve(lambda e: e.tensor_single_scalar(notm[:], posb[:], 0, ALU.not_equal), [kpb], [km])
    self.dve(lambda e: e.tensor_scalar(m0[:], notm[:], -1.0, 1.0, ALU.mult, ALU.add), [km], [km])
    hin = None
    if mode == "P2":
        csel = ar.t([128, 8]); kcs = self.key()
        self.load_bcast(csel[:], csel_d, kcs)
        la = ar.t([128, 8, 16]); kla = self.key()
        self.dma(la[:], lsum_all.rearrange("c p k -> p c k"), [], [kla])
        hin = ar.t([128, 8]); khin = self.key()
        fP = ar.t([128, 8]); hs = ar.t([128, 8])
        self.dve(lambda e: e.memset(hin[:], 0.0), [], [khin])
        for c in range(NCORES):
            self.dve(lambda e, c=c: e.tensor_scalar(fP[:], la[:, c, 0:8], -1.0, csel[:, c:c + 1], ALU.add, ALU.mult),
                     [kla, kcs], [khin])
            self.dve(lambda e: e.tensor_scalar(fP[:], fP[:], 1.0, None, ALU.add), [khin], [khin])
            self.dve(lambda e, c=c: e.tensor_scalar(hs[:], la[:, c, 8:16], csel[:, c:c + 1], None, ALU.mult),
                     [kla, kcs], [khin])
            self.dve(lambda e: e.tensor_tensor(hin[:], hin[:], fP[:], ALU.mult), [khin], [khin])
            self.dve(lambda e: e.tensor_tensor(hin[:], hin[:], hs[:], ALU.add), [khin], [khin])
    else:
        lsum = ar.t([128, 16]); kls = self.key()
        zeros = ar.t([128, T]); kz = self.key()
        self.dve(lambda e: e.memset(zeros[:], 0.0), [], [kz])
    xin = ar.t([128, T + 4]); kxin = self.key()
    xc = ar.t([128, T]); kxc = self.key()
    gr = ar.t([128, T]); kgr = self.key()
    gi = ar.t([128, T]); kgi = self.key()
    a = ar.t([128, T]); ka = self.key()
    mu = ar.t([128, T]); kmu = self.key()
    bb = ar.t([128, T]); kbb = self.key()
    hh = ar.t([128, T]); khh = self.key()
    if mode == "P2":
        yb = ar.t([128, T]); kyb = self.key()
        t1 = ar.t([128, T]); kt1 = self.key()
    psi = 0
    NTB = T // 512 if T >= 512 else 1
    TB = min(512, T)
    for cb in range(8):
        self.dma(xin[:, 0:3], zh[cb], [], [kxin], slow=True)
        self.dma(xin[:, 3:3 + T], zfm[cb], [], [kxin])
        self.dve(lambda e, cb=cb: e.tensor_scalar(xc[:], xin[:, 0:T], cw[:, 0, cb:cb + 1], cb_[:, cb:cb + 1],
                                                  ALU.mult, ALU.add), [kxin, kc_], [kxc])
        for j in range(1, 4):
            self.dve(lambda e, cb=cb, j=j: e.scalar_tensor_tensor(xc[:], xin[:, j:j + T], cw[:, j, cb:cb + 1], xc[:],
                                                                  ALU.mult, ALU.add), [kxin, kc_, kxc], [kxc])
        for tb in range(NTB):
            for (wt, bt, go, kgo) in ((wa, ba, gr, kgr), (wx, bx, gi, kgi)):
                p = psi % 8
                psi += 1
                self.mm(self.ps[p][:, 0:TB], wt[:, cb, :], xc[:, tb * TB:(tb + 1) * TB], True, True,
                        [kw, kxc], [self.psk[p]])
                self.act(go[:, tb * TB:(tb + 1) * TB], self.ps[p][:, 0:TB], AF.Sigmoid, [self.psk[p], kc_], [kgo],
                         bias=bt[:, cb:cb + 1])
        self.act(a[:], gr[:], AF.Exp, [kgr, knsp], [ka], scale=nsp[:, cb:cb + 1])
        self.dve(lambda e: e.tensor_tensor(mu[:], a[:], a[:], ALU.mult), [ka], [kmu])
        self.dve(lambda e: e.tensor_scalar(mu[:], mu[:], -1.0, 1.0, ALU.mult, ALU.add), [kmu], [kmu])
        self.act(mu[:], mu[:], AF.Sqrt, [kmu], [kmu])
        self.dve(lambda e: e.tensor_tensor(a[:], a[:], notm[:], ALU.mult), [ka, km], [ka])
        self.dve(lambda e: e.tensor_tensor(mu[:], mu[:], notm[:], ALU.mult), [kmu, km], [kmu])
        self.dve(lambda e: e.tensor_tensor(mu[:], mu[:], m0[:], ALU.add), [kmu, km], [kmu])
        self.dve(lambda e: e.tensor_tensor(bb[:], xc[:], gi[:], ALU.mult), [kxc, kgi], [kbb])
        self.dve(lambda e: e.tensor_tensor(bb[:], bb[:], mu[:], ALU.mult), [kbb, kmu], [kbb])
        if mode == "P1":
            self.dve(lambda e: e.tensor_tensor_scan(hh[:], a[:], bb[:], 0.0, ALU.mult, ALU.add), [ka, kbb], [khh])
            self.dve(lambda e, cb=cb: e.tensor_copy(lsum[:, 8 + cb:9 + cb], hh[:, T - 1:T]), [khh], [kls])
            self.dve(lambda e: e.tensor_tensor_scan(hh[:], a[:], zeros[:], 1.0, ALU.mult, ALU.add), [ka, kz], [khh])
            self.dve(lambda e, cb=cb: e.tensor_copy(lsum[:, cb:cb + 1], hh[:, T - 1:T]), [khh], [kls])
        else:
            self.dve(lambda e, cb=cb: e.tensor_tensor_scan(hh[:], a[:], bb[:], hin[:, cb:cb + 1], ALU.mult, ALU.add),
                     [ka, kbb, khin], [khh])
            self.dma(yb[:], zfm[8 + cb], [], [kyb])
            gelu_tanh(self, yb[:], yb[:], t1[:], [kyb], kyb, kt1)
            self.dve(lambda e: e.tensor_tensor(hh[:], hh[:], yb[:], ALU.mult), [khh, kyb], [khh])
            self.dma(rnnT_d[cb], hh[:], [khh], [])
    if mode == "P1":
        self.dma(lsum_out, lsum[:], [kls], [])
    self.S.barrier()


def stage_D(self, retT_d, rnnT_d, zfm, w_ret, w_rnn, w_mix, x_src, x_dst):
    T, BLK = self.T, self.BLK
    NB = BLK // 128
    ar = self.ar
    ar.reset()
    Wr = ar.t([128, 8, 1024]); Wn = ar.t([128, 8, 1024]); Wm = ar.t([128, 8, 1024]); kW = self.key()
    self.dma(Wr[:], w_ret.rearrange("(kc p) c -> p kc c", p=128), [], [kW])
    self.dma(Wn[:], w_rnn.rearrange("(kc p) c -> p kc c", p=128), [], [kW])
    self.dma(Wm[:], w_mix.rearrange("(kc p) c -> p kc c", p=128), [], [kW])
    rT = ar.t([128, 8, BLK]); nT = ar.t([128, 8, BLK]); kin = self.key()
    mT = ar.t([128, 8, BLK]); kmT = self.key()
    ga = [ar.t([128, BLK]) for _ in range(2)]; gb = [ar.t([128, BLK]) for _ in range(2)]
    kga = [self.key() for _ in range(2)]
    m1 = [ar.t([128, BLK]) for _ in range(2)]; km1 = [self.key() for _ in range(2)]
    xt = [ar.t([128, D]) for _ in range(2)]; kx = [self.key() for _ in range(2)]
    retT_v = retT_d.rearrange("c p t -> p c t")
    rnnT_v = rnnT_d.rearrange("c p t -> p c t")
    x_sv = x_src.rearrange("(n p) c -> n p c", p=128)
    x_dv = x_dst.rearrange("(n p) c -> n p c", p=128)
    psi = 0
    for blk in range(T // BLK):
        sl = slice(blk * BLK, (blk + 1) * BLK)
        self.dma(rT[:], retT_v[:, :, sl], [], [kin])
        self.dma(nT[:], rnnT_v[:, :, sl], [], [kin])
        for dc in range(8):
            b = dc % 2
            self.dma(ga[b][:], zfm[16 + dc][:, sl], [], [kga[b]])
            self.dma(gb[b][:], zfm[24 + dc][:, sl], [], [kga[b]])
            self.act(ga[b][:], ga[b][:], AF.Sigmoid, [kga[b]], [kga[b]])
            self.act(gb[b][:], gb[b][:], AF.Sigmoid, [kga[b]], [kga[b]])
            pr = psi % 8; psi += 1
            pn = psi % 8; psi += 1
            for kc in range(8):
                self.mm(self.ps[pr][:, 0:BLK], Wr[:, kc, dc * 128:(dc + 1) * 128], rT[:, kc, :], kc == 0, kc == 7,
                        [kW, kin], [self.psk[pr]])
            for kc in range(8):
                self.mm(self.ps[pn][:, 0:BLK], Wn[:, kc, dc * 128:(dc + 1) * 128], nT[:, kc, :], kc == 0, kc == 7,
                        [kW, kin], [self.psk[pn]])
            self.dve(lambda e, b=b, pr=pr: e.tensor_tensor(m1[b][:], self.ps[pr][:, 0:BLK], ga[b][:], ALU.mult),
                     [self.psk[pr], kga[b]], [km1[b]])
            self.dve(lambda e, b=b, pn=pn: e.tensor_tensor(gb[b][:], self.ps[pn][:, 0:BLK], gb[b][:], ALU.mult),
                     [self.psk[pn], kga[b]], [kga[b]])
            self.dve(lambda e, b=b, dc=dc: e.tensor_tensor(mT[:, dc, :], m1[b][:], gb[b][:], ALU.add),
                     [km1[b], kga[b]], [kmT])
        for i in range(NB):
            tile = blk * NB + i
            b = tile % 2
            self.dma(xt[b][:], x_sv[tile], [], [kx[b]])
            for half in range(2):
                p = psi % 8; psi += 1
                for kc in range(8):
                    self.mm(self.ps[p][:], mT[:, kc, i * 128:(i + 1) * 128], Wm[:, kc, half * 512:(half + 1) * 512],
                            kc == 0, kc == 7, [kW, kmT], [self.psk[p]])
                self.dve(lambda e, b=b, p=p, half=half: e.tensor_tensor(
                    xt[b][:, half * 512:(half + 1) * 512], xt[b][:, half * 512:(half + 1) * 512], self.ps[p][:],
                    ALU.add), [kx[b], self.psk[p]], [kx[b]])
            self.dma(x_dv[tile], xt[b][:], [kx[b]], [])
    self.S.barrier()


K.stage_B1 = stage_B1
K.stage_B2 = stage_B2
K.stage_C = stage_C
K.stage_D = stage_D


def stage_E0(self, mem_d, g_mem, w_xk, w_xv, kT_d, v_d):
    ar = self.ar
    ar.reset()
    gbm = ar.t([128, D]); kg = self.key()
    self.load_bcast(gbm[:], g_mem, kg)
    Wk = ar.t([128, 8, 1024]); Wv = ar.t([128, 8, 1024]); kW = self.key()
    self.dma(Wk[:], w_xk.rearrange("(kc p) c -> p kc c", p=128), [], [kW])
    self.dma(Wv[:], w_xv.rearrange("(kc p) c -> p kc c", p=128), [], [kW])
    mT = ar.t([128, 8, 256]); kmT = self.key()
    xt = ar.t([128, D]); kx = self.key()
    hb = ar.t([128, D]); kh = self.key()
    junk = ar.t([128, D]); kj = self.key()
    ss = ar.t([128, 4]); kss = self.key()
    st = [ar.t([128, 512]) for _ in range(2)]; kst = [self.key() for _ in range(2)]
    mem_v = mem_d.rearrange("(n p) c -> n p c", p=128)
    v_v = v_d.rearrange("(n p) c -> n p c", p=128)
    psi = 0
    for mt in range(2):
        self.dma(xt[:], mem_v[mt], [], [kx])
        self.rmsnorm_rows(xt, kx, gbm, kg, hb, kh, ss, kss, junk, kj)
        for half in range(2):
            p = psi % 8; psi += 1
            for j in range(4):
                kc = half * 4 + j
                self.tr(self.ps[p][:, j * 128:(j + 1) * 128], hb[:, kc * 128:(kc + 1) * 128], self.ident[:],
                        [kh, self.kI], [self.psk[p]])
            self.act(mT[:, half * 4:(half + 1) * 4, mt * 128:(mt + 1) * 128],
                     self.ps[p][:].rearrange("p (j c) -> p j c", c=128), AF.Copy, [self.psk[p]], [kmT])
    si = 0
    for dc in range(8):
        p = psi % 8; psi += 1
        for kc in range(8):
            self.mm(self.ps[p][:, 0:256], Wk[:, kc, dc * 128:(dc + 1) * 128], mT[:, kc, :], kc == 0, kc == 7,
                    [kW, kmT], [self.psk[p]])
        b = si % 2; si += 1
        self.act(st[b][:, 0:256], self.ps[p][:, 0:256], AF.Copy, [self.psk[p]], [kst[b]])
        self.dma(kT_d[dc], st[b][:, 0:256], [kst[b]], [])
    for mt in range(2):
        for half in range(2):
            p = psi % 8; psi += 1
            for kc in range(8):
                self.mm(self.ps[p][:], mT[:, kc, mt * 128:(mt + 1) * 128], Wv[:, kc, half * 512:(half + 1) * 512],
                        kc == 0, kc == 7, [kW, kmT], [self.psk[p]])
            b = si % 2; si += 1
            self.act(st[b][:], self.ps[p][:], AF.Copy, [self.psk[p]], [kst[b]])
            self.dma(v_v[mt][:, half * 512:(half + 1) * 512], st[b][:], [kst[b]], [])
    self.S.barrier()


def stage_E(self, x_src, x_dst, g_x, w_xq, w_xo, kT_d, v_d):
    T, BLK = self.T, self.BLK
    NB = BLK // 128
    ar = self.ar
    ar.reset()
    gbx = ar.t([128, D]); kg = self.key()
    self.load_bcast(gbx[:], g_x, kg)
    Wq = ar.t([128, 8, 1024]); Wo = ar.t([128, 8, 1024]); kW = self.key()
    self.dma(Wq[:], w_xq.rearrange("(kc p) c -> p kc c", p=128), [], [kW])
    self.dma(Wo[:], w_xo.rearrange("(kc p) c -> p kc c", p=128), [], [kW])
    kT = ar.t([128, 8, 256]); vv = ar.t([128, 2, 1024]); kkv = self.key()
    self.dma(kT[:], kT_d.rearrange("c p m -> p c m"), [], [kkv])
    self.dma(vv[:], v_d.rearrange("(n p) c -> p n c", p=128), [], [kkv])
    xt = [ar.t([128, D]) for _ in range(NB)]; kx = [self.key() for _ in range(NB)]
    hb = ar.t([128, D]); kh = self.key()
    junk = ar.t([128, D]); kj = self.key()
    ss = ar.t([128, 4]); kss = self.key()
    h2T = ar.t([128, 8, BLK]); kh2 = self.key()
    qT = ar.t([128, 8, BLK]); kqT = self.key()
    oT = ar.t([128, 8, BLK]); koT = self.key()
    pb = [ar.t([128, 256]) for _ in range(2)]; kpb = [self.key() for _ in range(2)]
    pT = [ar.t([128, 2, 128]) for _ in range(2)]; kpT = [self.key() for _ in range(2)]
    sm = ar.t([128, 16]); ksm = self.key()
    x_sv = x_src.rearrange("(n p) c -> n p c", p=128)
    x_dv = x_dst.rearrange("(n p) c -> n p c", p=128)
    psi = 0
    hi = 0

    def nb():
        nonlocal psi
        p = psi % 8
        psi += 1
        return p

    for blk in range(T // BLK):
        for i in range(NB):
            tile = blk * NB + i
            self.dma(xt[i][:], x_sv[tile], [], [kx[i]])
            self.rmsnorm_rows(xt[i], kx[i], gbx, kg, hb, kh, ss, kss, junk, kj)
            for half in range(2):
                p = nb()
                for j in range(4):
                    kc = half * 4 + j
                    self.tr(self.ps[p][:, j * 128:(j + 1) * 128], hb[:, kc * 128:(kc + 1) * 128], self.ident[:],
                            [kh, self.kI], [self.psk[p]])
                self.act(h2T[:, half * 4:(half + 1) * 4, i * 128:(i + 1) * 128],
                         self.ps[p][:].rearrange("p (j c) -> p j c", c=128), AF.Copy, [self.psk[p]], [kh2])
        for dc in range(8):
            p = nb()
            for kc in range(8):
                self.mm(self.ps[p][:, 0:BLK], Wq[:, kc, dc * 128:(dc + 1) * 128], h2T[:, kc, :], kc == 0, kc == 7,
                        [kW, kh2], [self.psk[p]])
            self.act(qT[:, dc, :], self.ps[p][:, 0:BLK], AF.Copy, [self.psk[p]], [kqT], scale=1.0 / 16.0)
        for i in range(NB):
            cs = slice(i * 128, (i + 1) * 128)
            for h in range(4):
                p = nb()
                sc = self.ps[p][:, 0:256]
                self.mm(sc, qT[:, 2 * h, cs], kT[:, 2 * h, :], True, False, [kqT, kkv], [self.psk[p]])
                self.mm(sc, qT[:, 2 * h + 1, cs], kT[:, 2 * h + 1, :], False, True, [kqT, kkv], [self.psk[p]])
                b = hi % 2
                hi += 1
                self.dve(lambda e, sc=sc, h=h: e.tensor_reduce(sm[:, h:h + 1], sc, axis=AX.X, op=ALU.max),
                         [self.psk[p]], [ksm])
                self.dve(lambda e, h=h: e.tensor_scalar(sm[:, 4 + h:5 + h], sm[:, h:h + 1], -1.0, None, ALU.mult),
                         [ksm], [ksm])
                self.act(pb[b][:], sc, AF.Exp, [self.psk[p], ksm], [kpb[b], ksm], bias=sm[:, 4 + h:5 + h],
                         accum_out=sm[:, 8 + h:9 + h])
                self.dve(lambda e, h=h: e.reciprocal(sm[:, 12 + h:13 + h], sm[:, 8 + h:9 + h]), [ksm], [ksm])
                self.dve(lambda e, h=h, b=b: e.tensor_scalar(pb[b][:], pb[b][:], sm[:, 12 + h:13 + h], None, ALU.mult),
                         [ksm, kpb[b]], [kpb[b]])
                p2 = nb()
                for mt in range(2):
                    self.tr(self.ps[p2][:, mt * 128:(mt + 1) * 128], pb[b][:, mt * 128:(mt + 1) * 128], self.ident[:],
                            [kpb[b], self.kI], [self.psk[p2]])
                self.act(pT[b][:].rearrange("p m t -> p (m t)"), self.ps[p2][:, 0:256], AF.Copy, [self.psk[p2]], [kpT[b]])
                p3 = nb()
                for e2 in range(2):
                    dc = 2 * h + e2
                    for mt in range(2):
                        self.mm(self.ps[p3][:, e2 * 128:(e2 + 1) * 128], vv[:, mt, dc * 128:(dc + 1) * 128], pT[b][:, mt, :],
                                mt == 0, mt == 1, [kkv, kpT[b]], [self.psk[p3]])
                self.act(oT[:, 2 * h:2 * h + 2, cs], self.ps[p3][:, 0:256].rearrange("p (e t) -> p e t", e=2), AF.Copy,
                         [self.psk[p3]], [koT])
        for i in range(NB):
            tile = blk * NB + i
            cs = slice(i * 128, (i + 1) * 128)
            for half in range(2):
                p = nb()
                for kc in range(8):
                    self.mm(self.ps[p][:], oT[:, kc, cs], Wo[:, kc, half * 512:(half + 1) * 512], kc == 0, kc == 7,
                            [kW, koT], [self.psk[p]])
                self.dve(lambda e, i=i, p=p, half=half: e.tensor_tensor(
                    xt[i][:, half * 512:(half + 1) * 512], xt[i][:, half * 512:(half + 1) * 512], self.ps[p][:],
                    ALU.add), [kx[i], self.psk[p]], [kx[i]])
            self.dma(x_dv[tile], xt[i][:], [kx[i]], [])
    self.S.barrier()


def top16(self, vals, idx, src, tmp, reads, kv, ktmp):
    self.dve(lambda e: e.max(vals[:, 0:8], src), reads, [kv])
    self.dve(lambda e: e.max_index(idx[:, 0:8], vals[:, 0:8], src), list(reads) + [kv], [kv])
    self.dve(lambda e: e.match_replace(tmp, vals[:, 0:8], src, -1e30), list(reads) + [kv], [ktmp])
    self.dve(lambda e: e.max(vals[:, 8:16], tmp), [ktmp], [kv])
    self.dve(lambda e: e.max_index(idx[:, 8:16], vals[:, 8:16], tmp), [ktmp, kv], [kv])


def stage_F(self, x_src, x_dst, g_ffn, w_pq, sk1_d, sk2_d, pu, pv, final, g_final=None, outn=None):
    T, NT = self.T, self.NT
    BLK = min(256, T)
    NB = BLK // 128
    ar = self.ar
    ar.reset()
    gbf = ar.t([128, D]); kg = self.key()
    self.load_bcast(gbf[:], g_ffn, kg)
    if final:
        gfin = ar.t([128, D]); kgf = self.key()
        self.load_bcast(gfin[:], g_final, kgf)
    Wp = ar.t([128, 8, 2048]); kW = self.key()
    self.dma(Wp[:, :, 0:1024], w_pq.rearrange("(kc p) c -> p kc c", p=128)[:, :, 0:1024], [], [kW])
    self.dma(Wp[:, :, 1024:2048], w_pq.rearrange("(kc p) c -> p kc c", p=128)[:, :, 1024:2048], [], [kW])
    skT = ar.t([128, 16, 128]); kskT = self.key()
    NG = 8
    gbuf = [ar.t([128, D]) for _ in range(NG)]; kgb = [self.key() for _ in range(NG)]
    skl = [gbuf[j][:].rearrange("p (h d) -> p h d", h=8) for j in range(2)]; kskl = self.key()
    self.dma(skl[0], sk1_d.rearrange("h n d -> n h d"), [], [kgb[0]])
    self.dma(skl[1], sk2_d.rearrange("h n d -> n h d"), [], [kgb[1]])
    psi = 0

    def nb():
        nonlocal psi
        p = psi % 8
        psi += 1
        return p

    for half in range(2):
        for hg in range(2):
            p = nb()
            for j in range(4):
                h = hg * 4 + j
                self.tr(self.ps[p][:, j * 128:(j + 1) * 128], skl[half][:, h, :], self.ident[:], [kgb[half], self.kI],
                        [self.psk[p]])
            for j in range(4):
                h = hg * 4 + j
                self.act(skT[:, 2 * h + half, :], self.ps[p][:, j * 128:(j + 1) * 128], AF.Copy, [self.psk[p]], [kskT])
    iota16 = ar.t([128, 16]); kio = self.key()
    self.pool(lambda e: e.iota(iota16[:], pattern=[[1, 16]], base=0, channel_multiplier=0, allow_small_or_imprecise_dtypes=True), [], [kio])
    xt = [ar.t([128, D]) for _ in range(NB)]; kx = [self.key() for _ in range(NB)]
    h3 = [ar.t([128, D]) for _ in range(NB)]; kh3 = [self.key() for _ in range(NB)]
    junk = ar.t([128, D]); kj = self.key()
    ss = ar.t([128, 4]); kss = self.key()
    h3T = ar.t([128, 8, BLK]); kh3T = self.key()
    qT = ar.t([128, 16, BLK]); kqT = self.key()
    s_all = ar.t([128, 16, 128]); ksa = self.key()
    tmpN = ar.t([128, 256]); ktmp = self.key()
    v12 = ar.t([128, 16, 16]); i12 = ar.t([128, 16, 16], U32); kv12 = self.key()
    i12f = ar.t([128, 16, 16]); ki12f = self.key()
    cand = ar.t([128, 8, 256]); kcand = self.key()
    cv = ar.t([128, 8, 16]); ci = ar.t([128, 8, 16], U32); kcv = self.key()
    ai = ar.t([128, 8, 16], I32); bi = ar.t([128, 8, 16], I32); af = ar.t([128, 8, 16]); bf = ar.t([128, 8, 16])
    kab = self.key()
    oh = ar.t([128, 8, 16, 16]); koh = self.key()
    e1 = ar.t([128, 8, 16]); e2 = ar.t([128, 8, 16]); ke = self.key()
    idxf = ar.t([128, 128]); idxi = ar.t([128, 128], I32); kidx = self.key()
    gates = ar.t([128, 8, 16]); gs = ar.t([128, 8]); kgt = self.key()
    actv = ar.t([128, 128]); kact = self.key()
    wgt = ar.t([128, 128]); t1 = ar.t([128, 128]); kwg = self.key(); kt1 = self.key()
    gi = 0
    if final:
        ss2 = ar.t([128, 4]); kss2 = self.key()
        ob = ar.t([128, D]); kob = self.key()
    x_sv = x_src.rearrange("(n p) c -> n p c", p=128)
    x_dv = x_dst.rearrange("(n p) c -> n p c", p=128)
    if final:
        outn_v = outn.rearrange("(n p) c -> n p c", p=128)

    def gather(tab, hk):
        nonlocal gi
        g = gi % NG
        gi += 1
        off = bass.IndirectOffsetOnAxis(ap=idxi[:, hk:hk + 1], axis=0)
        def f(e, g=g, off=off):
            if "r" not in self.regc:
                self.regc["r"] = e.to_reg(16383)
            return e.indirect_dma_start(out=gbuf[g][:], out_offset=None, in_=tab, in_offset=off,
                                        bounds_check=self.regc["r"], oob_is_err=False)
        self.S.op("pool", f, [kidx], [kgb[g]], dma=True)
        return g

    for blk in range(T // BLK):
        for i in range(NB):
            tile = blk * NB + i
            self.dma(xt[i][:], x_sv[tile], [], [kx[i]])
            self.rmsnorm_rows(xt[i], kx[i], gbf, kg, h3[i], kh3[i], ss, kss, junk, kj)
            for half in range(2):
                p = nb()
                for j in range(4):
                    kc = half * 4 + j
                    self.tr(self.ps[p][:, j * 128:(j + 1) * 128], h3[i][:, kc * 128:(kc + 1) * 128], self.ident[:],
                            [kh3[i], self.kI], [self.psk[p]])
                self.act(h3T[:, half * 4:(half + 1) * 4, i * 128:(i + 1) * 128],
                         self.ps[p][:].rearrange("p (j c) -> p j c", c=128), AF.Copy, [self.psk[p]], [kh3T])
        for c in range(16):
            p = nb()
            for kc in range(8):
                self.mm(self.ps[p][:, 0:BLK], Wp[:, kc, c * 128:(c + 1) * 128], h3T[:, kc, :], kc == 0, kc == 7,
                        [kW, kh3T], [self.psk[p]])
            self.act(qT[:, c, :], self.ps[p][:, 0:BLK], AF.Copy, [self.psk[p]], [kqT])
        for i in range(NB):
            tile = blk * NB + i
            cs = slice(i * 128, (i + 1) * 128)
            for jg in range(4):
                p = nb()
                for jj in range(4):
                    j = jg * 4 + jj
                    self.mm(self.ps[p][:, jj * 128:(jj + 1) * 128], qT[:, j, cs], skT[:, j, :], True, True,
                            [kqT, kskT], [self.psk[p]])
                self.act(s_all[:, jg * 4:(jg + 1) * 4, :], self.ps[p][:].rearrange("p (j c) -> p j c", c=128),
                         AF.Copy, [self.psk[p]], [ksa])
            for j in range(16):
                top16(self, v12[:, j, :], i12[:, j, :], s_all[:, j, :], tmpN[:, 0:128], [ksa], kv12, ktmp)
            self.dve(lambda e: e.tensor_copy(i12f[:], i12[:]), [kv12], [ki12f])
            v12v = v12[:].rearrange("p (h two) k -> p h two k", two=2)
            i12v = i12f[:].rearrange("p (h two) k -> p h two k", two=2)
            self.dve(lambda e, v12v=v12v: e.tensor_tensor(
                cand[:].rearrange("p h (a b) -> p h a b", b=16),
                v12v[:, :, 0, :].unsqueeze(3).broadcast_to([128, 8, 16, 16]),
                v12v[:, :, 1, :].unsqueeze(2).broadcast_to([128, 8, 16, 16]), ALU.add), [kv12], [kcand])
            for h in range(8):
                top16(self, cv[:, h, :], ci[:, h, :], cand[:, h, :], tmpN[:, 0:256], [kcand], kcv, ktmp)
            cii = ci[:].bitcast(I32)
            self.dve(lambda e, cii=cii: e.tensor_single_scalar(ai[:], cii, 4, ALU.logical_shift_right), [kcv], [kab])
            self.dve(lambda e, cii=cii: e.tensor_single_scalar(bi[:], cii, 15, ALU.bitwise_and), [kcv], [kab])
            self.dve(lambda e: e.tensor_copy(af[:], ai[:]), [kab], [kab])
            self.dve(lambda e: e.tensor_copy(bf[:], bi[:]), [kab], [kab])
            io_b = iota16[:].unsqueeze(1).unsqueeze(1).broadcast_to([128, 8, 16, 16])
            for (sel, tabv, eo) in ((af, 0, e1), (bf, 1, e2)):
                self.dve(lambda e, sel=sel, io_b=io_b: e.tensor_tensor(
                    oh[:], sel[:].unsqueeze(3).broadcast_to([128, 8, 16, 16]), io_b, ALU.is_equal),
                    [kab, kio], [koh])
                self.dve(lambda e, tabv=tabv, i12v=i12v: e.tensor_tensor(
                    oh[:], oh[:], i12v[:, :, tabv, :].unsqueeze(2).broadcast_to([128, 8, 16, 16]), ALU.mult),
                    [koh, ki12f], [koh])
                self.dve(lambda e, eo=eo: e.tensor_reduce(eo[:], oh[:], axis=AX.X, op=ALU.add), [koh], [ke])
            self.dve(lambda e: e.scalar_tensor_tensor(idxf[:], e1[:].rearrange("p h k -> p (h k)"), 128.0,
                                                      e2[:].rearrange("p h k -> p (h k)"), ALU.mult, ALU.add),
                     [ke], [kidx])
            self.dve(lambda e: e.tensor_scalar(idxf[:], idxf[:], 0.0, 16383.0, ALU.max, ALU.min), [kidx], [kidx])
            self.dve(lambda e: e.tensor_copy(idxi[:], idxf[:]), [kidx], [kidx])
            self.dve(lambda e: e.tensor_tensor(gates[:], cv[:], cv[:, :, 0:1].broadcast_to([128, 8, 16]), ALU.subtract),
                     [kcv], [kgt])
            self.act(gates[:], gates[:], AF.Exp, [kgt], [kgt])
            self.dve(lambda e: e.tensor_reduce(gs[:], gates[:], axis=AX.X, op=ALU.add), [kgt], [kgt])
            self.dve(lambda e: e.reciprocal(gs[:], gs[:]), [kgt], [kgt])
            self.dve(lambda e: e.tensor_tensor(gates[:], gates[:], gs[:].unsqueeze(2).broadcast_to([128, 8, 16]),
                                               ALU.mult), [kgt], [kgt])
            LOOK = NG - 2
            pend = []
            for hk in range(min(LOOK, 128)):
                pend.append((gather(pu, hk), hk))
            nxt = len(pend)
            for hk in range(128):
                g, hk_ = pend.pop(0)
                self.dve(lambda e, g=g, hk=hk, i=i: e.scalar_tensor_tensor(
                    junk[:], gbuf[g][:], 1.0, h3[i][:], ALU.mult, ALU.mult, accum_out=actv[:, hk:hk + 1]),
                    [kgb[g], kh3[i]], [kj, kact, kgb[g]])
                if nxt < 128:
                    pend.append((gather(pu, nxt), nxt))
                    nxt += 1
            gelu_tanh(self, wgt[:], actv[:], t1[:], [kact], kwg, kt1)
            self.dve(lambda e: e.tensor_tensor(wgt[:], wgt[:], gates[:].rearrange("p h k -> p (h k)"), ALU.mult),
                     [kwg, kgt], [kwg])
            pend = []
            for hk in range(min(LOOK, 128)):
                pend.append((gather(pv, hk), hk))
            nxt = len(pend)
            for hk in range(128):
                g, hk_ = pend.pop(0)
                self.dve(lambda e, g=g, hk=hk, i=i: e.scalar_tensor_tensor(
                    xt[i][:], gbuf[g][:], wgt[:, hk:hk + 1], xt[i][:], ALU.mult, ALU.add),
                    [kgb[g], kwg, kx[i]], [kx[i], kgb[g]])
                if nxt < 128:
                    pend.append((gather(pv, nxt), nxt))
                    nxt += 1
            self.dma(x_dv[tile], xt[i][:], [kx[i]], [])
            if final:
                self.rmsnorm_rows(xt[i], kx[i], gfin, kgf, ob, kob, ss2, kss2, junk, kj)
                self.dma(outn_v[tile], ob[:], [kob], [])
    self.S.barrier()


K.stage_E0 = stage_E0
K.stage_E = stage_E
K.stage_F = stage_F


def build_P1(T):
    k = K(T, "P1", False)
    x = k.inp("x", [T, D]); xh = k.inp("xh", [3, D]); w_in = k.inp("w_in", [D, 7168])
    g = k.inp("g_mix", [D]); pos = k.inp("pos", [T], I32); invf = k.inp("invf", [64])
    conv_w = k.inp("conv_w", [4, D]); conv_b = k.inp("conv_b", [D])
    wa = k.inp("lru_wa", [8, 128, 128]); ba = k.inp("lru_ba", [8, 128])
    wx = k.inp("lru_wx", [8, 128, 128]); bx = k.inp("lru_bx", [8, 128]); lam = k.inp("lru_lam", [D])
    send = k.outp("send", [128, 1024]); lsum = k.outp("lsum", [128, 16])
    ztm = k.scratch("ztm", [T, 3072]); zfm = k.scratch("zfm", [32, 128, T]); zh = k.scratch("zh", [8, 128, 3])
    k.consts()
    k.stage_A(x, xh, w_in, g, pos, invf, ztm, zfm, zh)
    k.stage_B1(ztm, send)
    k.stage_C(zfm, zh, pos, conv_w, conv_b, wa, ba, wx, bx, lam, "P1", lsum_out=lsum)
    k.S.emit()
    return k


def build_P2(T, final, dbg=False, stages="ABCDEF"):
    k = K(T, "P2", final)
    x = k.inp("x", [T, D]); xh = k.inp("xh", [3, D]); w_in = k.inp("w_in", [D, 7168])
    g = k.inp("g_mix", [D]); pos = k.inp("pos", [T], I32); invf = k.inp("invf", [64])
    conv_w = k.inp("conv_w", [4, D]); conv_b = k.inp("conv_b", [D])
    wa = k.inp("lru_wa", [8, 128, 128]); ba = k.inp("lru_ba", [8, 128])
    wx = k.inp("lru_wx", [8, 128, 128]); bx = k.inp("lru_bx", [8, 128]); lam = k.inp("lru_lam", [D])
    send_all = k.inp("send_all", [8, 128, 1024]); lsum_all = k.inp("lsum_all", [8, 128, 16]); csel = k.inp("csel", [8])
    w_ret = k.inp("w_ret_br", [D, D]); w_rnn = k.inp("w_rnn_br", [D, D]); w_mix = k.inp("w_mix_out", [D, D])
    mem = k.inp("mem", [256, D]); g_x = k.inp("g_x", [D]); g_mem = k.inp("g_mem", [D])
    w_xq = k.inp("w_xq", [D, D]); w_xk = k.inp("w_xk", [D, D]); w_xv = k.inp("w_xv", [D, D]); w_xo = k.inp("w_xo", [D, D])
    g_ffn = k.inp("g_ffn", [D]); w_pq = k.inp("w_pq", [D, 2048])
    sk1 = k.inp("sub_k1", [8, 128, 128]); sk2 = k.inp("sub_k2", [8, 128, 128])
    pu = k.inp("peer_u", [16384, D]); pv = k.inp("peer_v", [16384, D]); g_final = k.inp("g_final", [D])
    out = k.outp("out", [T, D]); outn = k.outp("outn", [T, D])
    mk = k.outp if dbg else k.scratch
    ztm = mk("ztm", [T, 3072]); zfm = mk("zfm", [32, 128, T]); zh = mk("zh", [8, 128, 3])
    retT = mk("retT", [8, 128, T]); rnnT = mk("rnnT", [8, 128, T])
    x1 = mk("x1", [T, D]); x2 = mk("x2", [T, D])
    kT = mk("kT", [8, 128, 256]); vm = mk("vm", [256, D])
    k.consts()
    k.stage_A(x, xh, w_in, g, pos, invf, ztm, zfm, zh)
    if "B" in stages:
        k.stage_B2(ztm, send_all, csel, retT)
    if "C" in stages:
        k.stage_C(zfm, zh, pos, conv_w, conv_b, wa, ba, wx, bx, lam, "P2", lsum_all=lsum_all, csel_d=csel, rnnT_d=rnnT)
    if "D" in stages:
        k.stage_D(retT, rnnT, zfm, w_ret, w_rnn, w_mix, x, x1)
    if "E" in stages:
        k.stage_E0(mem, g_mem, w_xk, w_xv, kT, vm)
        k.stage_E(x1, x2, g_x, w_xq, w_xo, kT, vm)
    if "F" in stages:
        k.stage_F(x2 if "E" in stages else x, out, g_ffn, w_pq, sk1, sk2, pu, pv, True, g_final, outn)
    k.S.emit()
    return k


_INVF = None


def _invf():
    return (10000.0 ** (-(np.arange(0, 128, 2, dtype=np.float32) / np.float32(128)))).astype(np.float32)


_PROGS = {}


def _prog(name, T, final=False):
    key = (name, T, final)
    if key not in _PROGS:
        _PROGS[key] = build_P1(T) if name == "P1" else build_P2(T, final)
    return _PROGS[key]


def run_model(inputs, ncores=NCORES, T=SEQ // NCORES, dbg=False):
    x = np.ascontiguousarray(inputs["x"][0])
    pos = np.ascontiguousarray(inputs["positions"][0]).astype(np.int32)
    mem = np.ascontiguousarray(inputs["mem"][0])
    invf = _invf()
    L = inputs["w_in"].shape[0]
    cids = list(range(ncores))
    for l in range(L):
        xs = [np.ascontiguousarray(x[c * T:(c + 1) * T]) for c in range(ncores)]
        xhs = [np.zeros((3, D), np.float32) if c == 0 else np.ascontiguousarray(x[c * T - 3:c * T])
               for c in range(ncores)]
        poss = [np.ascontiguousarray(pos[c * T:(c + 1) * T]) for c in range(ncores)]
        common1 = dict(w_in=inputs["w_in"][l], g_mix=inputs["g_mix"][l], invf=invf, conv_w=inputs["conv_w"][l],
                       conv_b=inputs["conv_b"][l], lru_wa=inputs["lru_wa"][l], lru_ba=inputs["lru_ba"][l],
                       lru_wx=inputs["lru_wx"][l], lru_bx=inputs["lru_bx"][l], lru_lam=inputs["lru_lam"][l])
        common1 = {k_: np.ascontiguousarray(v) for k_, v in common1.items()}
        k1 = _prog("P1", T)
        in1 = [dict(common1, x=xs[c], xh=xhs[c], pos=poss[c]) for c in range(ncores)]
        r1 = run_bass_kernel_spmd(k1.nc, in1, core_ids=cids).results
        send_all = np.zeros((8, 128, 1024), np.float32)
        lsum_all = np.zeros((8, 128, 16), np.float32)
        for c in range(ncores):
            send_all[c] = r1[c]["send"]
            lsum_all[c] = r1[c]["lsum"]
        final = (l == L - 1)
        k2 = _prog("P2", T, True) if not dbg else build_P2(T, final, dbg=True, stages=dbg if isinstance(dbg, str) else "ABCDEF")
        common2 = dict(common1, send_all=send_all, lsum_all=lsum_all, w_ret_br=inputs["w_ret_br"][l],
                       w_rnn_br=inputs["w_rnn_br"][l], w_mix_out=inputs["w_mix_out"][l], mem=mem,
                       g_x=inputs["g_x"][l], g_mem=inputs["g_mem"][l], w_xq=inputs["w_xq"][l], w_xk=inputs["w_xk"][l],
                       w_xv=inputs["w_xv"][l], w_xo=inputs["w_xo"][l], g_ffn=inputs["g_ffn"][l], w_pq=inputs["w_pq"][l],
                       sub_k1=inputs["sub_k1"][l], sub_k2=inputs["sub_k2"][l], peer_u=inputs["peer_u"][l],
                       peer_v=inputs["peer_v"][l], g_final=inputs["g_final"])
        common2 = {k_: np.ascontiguousarray(v) for k_, v in common2.items()}
        in2 = []
        for c in range(ncores):
            cs = np.zeros(8, np.float32)
            cs[:c] = 1.0
            in2.append(dict(common2, x=xs[c], xh=xhs[c], pos=poss[c], csel=cs))
        r2 = run_bass_kernel_spmd(k2.nc, in2, core_ids=cids).results
        x = np.concatenate([r2[c]["out"] for c in range(ncores)], axis=0)
        if dbg:
            return r1, r2, x
    return np.concatenate([r2[c]["outn"] for c in range(ncores)], axis=0)


def kernel(**inputs):
    out = run_model(inputs)
    return out[None].astype(np.float32)
```

```python
import math
import numpy as np
import concourse.bass as bass
import concourse.mybir as mybir
from concourse.bass_utils import run_bass_kernel_spmd

F32 = mybir.dt.float32
I32 = mybir.dt.int32
U32 = mybir.dt.uint32
ALU = mybir.AluOpType
AF = mybir.ActivationFunctionType
AX = mybir.AxisListType

D = 1024
NCORES = 8
SEQ = 16384
DEPTH = 2
EPS = 1e-6
ENGS = ("pe", "act", "dve", "pool", "sp")
EPOCH = 3000


class Sched:
    def __init__(self, nc, sems):
        self.nc = nc
        self.free = list(sems)
        self.streams = {e: [] for e in ENGS}
        self.cur = {}
        self.seen = {e: {} for e in ENGS}
        self.lastw = {}
        self.readers = {}
        self.latest = {}
        nd = {"sp": 24, "pool": 40}
        self.dma_sems = {q: [self.free.pop() for _ in range(n)] for q, n in nd.items()}
        self.dma_rr = {"sp": 0, "pool": 0}
        self.dma_use = {}
        self.nops = 0

    def _tok(self, eng):
        c = self.cur.get(eng)
        if c is None or c[1] >= EPOCH:
            c = [self.free.pop(), 0]
            self.cur[eng] = c
        c[1] += 1
        return (c[0], c[1])

    def op(self, eng, fn, reads=(), writes=(), dma=False):
        waits = {}
        seen = self.seen[eng]

        def need(tok):
            if tok is None:
                return
            s, v = tok
            if seen.get(s, 0) >= v:
                return
            if waits.get(s, 0) < v:
                waits[s] = v

        for k in reads:
            need(self.lastw.get(k))
        for k in writes:
            need(self.lastw.get(k))
            for s, v in self.readers.get(k, {}).items():
                need((s, v))
        if dma == "cc":
            s = self.free.pop()
            tok = (s, 1)
            inc = (s, 1)
        elif dma:
            sl = self.dma_sems[eng]
            s = sl[self.dma_rr[eng] % len(sl)]
            self.dma_rr[eng] += 1
            prev = self.dma_use.get(s, 0)
            need((s, prev))
            tok = (s, prev + 16)
            self.dma_use[s] = prev + 16
            inc = (s, 16)
        else:
            tok = self._tok(eng)
            inc = (tok[0], 1)
            if eng == "pe":
                seen[tok[0]] = tok[1]
        for s, v in waits.items():
            seen[s] = v
        self.latest[tok[0]] = tok[1]
        for k in reads:
            r = self.readers.setdefault(k, {})
            if r.get(tok[0], 0) < tok[1]:
                r[tok[0]] = tok[1]
        for k in writes:
            self.lastw[k] = tok
            self.readers[k] = {}
        self.streams[eng].append((list(waits.items()), fn, inc))
        self.nops += 1
        return tok

    def barrier(self):
        for e in ENGS:
            waits = []
            for s, v in self.latest.items():
                if self.seen[e].get(s, 0) < v:
                    waits.append((s, v))
                    self.seen[e][s] = v
            if waits:
                self.streams[e].append((waits, None, None))
        self.lastw = {}
        self.readers = {}

    def emit(self):
        nc = self.nc

        def run(e, name):
            for waits, fn, inc in self.streams[name]:
                for s, v in waits:
                    e.wait_ge(s, v)
                if fn is not None:
                    ins = fn(e)
                    ins.then_inc(inc[0], inc[1])

        with nc.Block() as block:
            @block.sync
            def _(e):
                run(e, "sp")

            @block.tensor
            def _(e):
                run(e, "pe")

            @block.vector
            def _(e):
                run(e, "dve")

            @block.scalar
            def _(e):
                run(e, "act")

            @block.gpsimd
            def _(e):
                run(e, "pool")


class Arena:
    def __init__(self, nc, base, limit):
        self.nc, self.base, self.limit = nc, base, limit
        self.ptr = base
        self.n = 0

    def reset(self):
        self.ptr = self.base

    def keep(self):
        self.base = self.ptr

    def t(self, shape, dtype=F32):
        nbytes = 4
        for s in shape[1:]:
            nbytes *= s
        self.n += 1
        off = (self.ptr + 63) // 64 * 64
        assert off + nbytes <= self.limit, (off, nbytes, self.limit)
        h = self.nc.alloc_sbuf_tensor_at(f"sb{self.n}", list(shape), dtype, offset=off)
        self.ptr = off + nbytes
        return h


class K:
    def __init__(self, T, phase, last):
        self.T, self.phase, self.last = T, phase, last
        self.NT = T // 128
        self.BLK = min(512, T)
        nc = bass.Bass("TRN2", target_bir_lowering=False)
        self.nc = nc
        self.stack = []
        sems = []
        import contextlib
        self.es = contextlib.ExitStack()
        for i in range(96):
            sems.append(self.es.enter_context(nc.semaphore(f"s{i}")))
        self.S = Sched(nc, sems)
        self.ar = Arena(nc, 16384, 16384 + 212000)
        self.ps = [nc.alloc_psum_tensor(f"ps{i}", [128, 512], F32) for i in range(8)]
        self.psk = [("ps", i) for i in range(8)]
        self.uid = 0
        self.regc = {}
        self.din = {}
        self.dout = {}

    def inp(self, name, shape, dtype=F32):
        a = self.nc.dram_tensor(name, list(shape), dtype, kind="ExternalInput").ap()
        self.din[name] = a
        return a

    def outp(self, name, shape, dtype=F32):
        a = self.nc.dram_tensor(name, list(shape), dtype, kind="ExternalOutput").ap()
        self.dout[name] = a
        return a

    def scratch(self, name, shape, dtype=F32):
        return self.nc.dram_tensor(name, list(shape), dtype).ap()

    def key(self, base="b"):
        self.uid += 1
        return (base, self.uid)

    def dma(self, out, in_, reads=(), writes=(), q="sp", slow=False):
        if slow:
            return self.S.op(q, lambda e: e.dma_start(out=out, in_=in_, allow_slow_non_contiguous=True),
                             reads, writes, dma=True)
        return self.S.op(q, lambda e: e.dma_start(out=out, in_=in_), reads, writes, dma=True)

    def mm(self, out, lhsT, rhs, start, stop, reads, writes):
        return self.S.op("pe", lambda e: e.matmul(out, lhsT, rhs, start=start, stop=stop), reads, writes)

    def tr(self, out, in_, ident, reads, writes):
        return self.S.op("pe", lambda e: e.transpose(out, in_, ident), reads, writes)

    def act(self, out, in_, func, reads, writes, bias=0.0, scale=1.0, accum_out=None):
        if accum_out is None:
            f = lambda e: e.activation(out, in_, func, bias=bias, scale=scale)
        else:
            f = lambda e: e.activation(out, in_, func, bias=bias, scale=scale, accum_out=accum_out)
        return self.S.op("act", f, reads, writes)

    def dve(self, f, reads, writes):
        return self.S.op("dve", f, reads, writes)

    def pool(self, f, reads, writes):
        return self.S.op("pool", f, reads, writes)

    def consts(self):
        ar = self.ar
        self.ident = ar.t([128, 128])
        kI = self.kI = ("ident",)
        ones = ar.t([128, 128])
        kO = self.key()
        self.pool(lambda e: e.memset(ones[:], 1.0), [], [kO])
        self.pool(lambda e: e.affine_select(self.ident[:], ones[:], pattern=[[-1, 128]],
                                            compare_op=ALU.is_equal, fill=0.0, base=0,
                                            channel_multiplier=1), [kO], [kI])
        self.ones = ones
        self.kOnes = kO
        ar.keep()

    def rmsnorm_rows(self, xt, kx, gb, kg, h, kh, ss, kss, junk, kj, rows=128):
        self.act(junk[0:rows, :], xt[0:rows, :], AF.Square, [kx], [kj, kss], accum_out=ss[0:rows, 0:1])
        self.dve(lambda e: e.tensor_scalar(ss[0:rows, 1:2], ss[0:rows, 0:1], 1.0 / D, EPS, ALU.mult, ALU.add),
                 [kss], [kss])
        self.act(ss[0:rows, 3:4], ss[0:rows, 1:2], AF.Sqrt, [kss], [kss])
        self.dve(lambda e: e.reciprocal(ss[0:rows, 2:3], ss[0:rows, 3:4]), [kss], [kss])
        self.dve(lambda e: e.scalar_tensor_tensor(h[0:rows, :], xt[0:rows, :], ss[0:rows, 2:3], gb[0:rows, :],
                                                  ALU.mult, ALU.mult), [kx, kss, kg], [kh])

    def load_bcast(self, dst, dram_vec, kd):
        self.dma(dst, dram_vec.partition_broadcast(128), [], [kd])

    def stage_A(self, x_d, xh_d, w_in, g_mix, pos_d, invf_d, ztm, zfm, zh):
        T, NT, BLK = self.T, self.NT, self.BLK
        ar = self.ar
        ar.reset()
        S = self.S
        gb = ar.t([128, D]); kg = self.key()
        self.load_bcast(gb[:], g_mix, kg)
        NF = 64
        posi = ar.t([128, NT], I32); kp = self.key()
        self.dma(posi[:], pos_d.rearrange("(n p) -> p n", p=128), [], [kp], slow=True)
        posf = ar.t([128, NT]); kpf = self.key()
        self.dve(lambda e: e.tensor_copy(posf[:], posi[:]), [kp], [kpf])
        invf = ar.t([128, NF]); kiv = self.key()
        self.load_bcast(invf[:], invf_d, kiv)
        ang = ar.t([128, NT, NF]); ka = self.key()
        self.dve(lambda e: e.tensor_tensor(ang[:], posf[:].unsqueeze(2).broadcast_to([128, NT, NF]),
                                           invf[:].unsqueeze(1).broadcast_to([128, NT, NF]), ALU.mult),
                 [kpf, kiv], [ka])
        kq = ar.t([128, NT, NF]); kkq = self.key()
        kqi = ar.t([128, NT, NF], I32); kkqi = self.key()
        TWO_PI = 2.0 * math.pi
        self.dve(lambda e: e.tensor_scalar(kq[:], ang[:], 1.0 / TWO_PI, None, ALU.mult), [ka], [kkq])
        self.dve(lambda e: e.tensor_copy(kqi[:], kq[:]), [kkq], [kkqi])
        self.dve(lambda e: e.tensor_copy(kq[:], kqi[:]), [kkqi], [kkq])
        C1 = 6.28125
        c2 = TWO_PI - C1
        C2 = float(np.float32(np.round(c2 * 2 ** 20) / 2 ** 20))
        C3 = float(np.float32(c2 - C2))
        r = ar.t([128, NT, NF]); kr = self.key()
        self.dve(lambda e: e.scalar_tensor_tensor(r[:], kq[:], -C1, ang[:], ALU.mult, ALU.add), [kkq, ka], [kr])
        self.dve(lambda e: e.scalar_tensor_tensor(r[:], kq[:], -C2, r[:], ALU.mult, ALU.add), [kkq, kr], [kr])
        self.dve(lambda e: e.scalar_tensor_tensor(r[:], kq[:], -C3, r[:], ALU.mult, ALU.add), [kkq, kr], [kr])
        tmp = ar.t([128, NT, NF]); kt = self.key()

        def wrap(buf, kb):
            self.dve(lambda e: e.tensor_scalar(tmp[:], buf[:], math.pi, -TWO_PI, ALU.is_gt, ALU.mult), [kb], [kt])
            self.dve(lambda e: e.tensor_tensor(buf[:], buf[:], tmp[:], ALU.add), [kb, kt], [kb])
            self.dve(lambda e: e.tensor_scalar(tmp[:], buf[:], -math.pi, TWO_PI, ALU.is_lt, ALU.mult), [kb], [kt])
            self.dve(lambda e: e.tensor_tensor(buf[:], buf[:], tmp[:], ALU.add), [kb, kt], [kb])
            self.dve(lambda e: e.tensor_scalar(buf[:], buf[:], math.pi, -math.pi, ALU.min, ALU.max), [kb], [kb])

        wrap(r, kr)
        r2 = ar.t([128, NT, NF]); kr2 = self.key()
        self.dve(lambda e: e.tensor_scalar(r2[:], r[:], math.pi / 2, None, ALU.add), [kr], [kr2])
        wrap(r2, kr2)
        sin_t = ar.t([128, NT, NF]); cos_t = ar.t([128, NT, NF]); ksc = self.key()
        self.act(sin_t[:], r[:], AF.Sin, [kr], [ksc])
        self.act(cos_t[:], r2[:], AF.Sin, [kr2], [ksc])
        sink_t = ar.t([128, NT, NF]); cosk_t = ar.t([128, NT, NF]); ksck = self.key()
        sc = 128 ** -0.5
        self.dve(lambda e: e.tensor_scalar(sink_t[:], sin_t[:], sc, None, ALU.mult), [ksc], [ksck])
        self.dve(lambda e: e.tensor_scalar(cosk_t[:], cos_t[:], sc, None, ALU.mult), [ksc], [ksck])

        NB = BLK // 128
        hT = ar.t([128, 8, BLK]); khT = [self.key() for _ in range(NB)]
        xt = [ar.t([128, D]) for _ in range(2)]; kx = [self.key() for _ in range(2)]
        hb = [ar.t([128, D]) for _ in range(2)]; kh = [self.key() for _ in range(2)]
        junk = ar.t([128, D]); kj = self.key()
        ss = [ar.t([128, 4]) for _ in range(2)]; kss = [self.key() for _ in range(2)]
        W = [ar.t([128, 8, 512]) for _ in range(2)]; kW = [self.key() for _ in range(2)]
        st = [ar.t([128, 512]) for _ in range(4)]; kst = [self.key() for _ in range(4)]
        rt = [ar.t([128, 4, 4, 64]) for _ in range(2)]; krt = [self.key() for _ in range(2)]
        hhT = ar.t([128, 8, 4]); khh = self.key()
        sth = ar.t([128, 8, 4]); ksth = self.key()
        w_in_v = w_in.rearrange("(kc p) c -> p kc c", p=128)
        x_v = x_d.rearrange("(n p) c -> n p c", p=128)
        ztm_v = ztm.rearrange("(n p) c -> n p c", p=128)
        psi = 0
        wi = 0
        sti = 0
        ti = 0

        def norm_T(src_ap, rows, dstT_fn, kdst):
            nonlocal ti, psi
            b = ti % 2
            ti += 1
            self.dma(xt[b][0:rows, :], src_ap, [], [kx[b]])
            self.rmsnorm_rows(xt[b], kx[b], gb, kg, hb[b], kh[b], ss[b], kss[b], junk, kj, rows=rows)
            for half in range(2):
                p = psi % 8
                psi += 1
                for j in range(4):
                    kc = half * 4 + j
                    self.tr(self.ps[p][:, j * 128:j * 128 + rows], hb[b][0:rows, kc * 128:(kc + 1) * 128],
                            self.ident[0:rows, 0:rows], [kh[b], self.kI], [self.psk[p]])
                dstT_fn(half, p)

        if xh_d is not None:
            def dst_h(half, p):
                self.act(hhT[:, half * 4:(half + 1) * 4, 0:3],
                         self.ps[p][:].rearrange("p (j c) -> p j c", c=128)[:, :, 0:3], AF.Copy,
                         [self.psk[p]], [khh])
            norm_T(xh_d, 3, dst_h, khh)

        for blk in range(T // BLK):
            for i in range(NB):
                tile = blk * NB + i

                def dst_t(half, p, i=i):
                    self.act(hT[:, half * 4:(half + 1) * 4, i * 128:(i + 1) * 128],
                             self.ps[p][:].rearrange("p (j c) -> p j c", c=128), AF.Copy,
                             [self.psk[p]], [khT[i]])
                norm_T(x_v[tile], 128, dst_t, khT[i])
            for grp in range(14):
                wb = wi % 2
                wi += 1
                self.dma(W[wb][:], w_in_v[:, :, grp * 512:(grp + 1) * 512], [], [kW[wb]])
                if grp < 6:
                    for i in range(NB):
                        tile = blk * NB + i
                        p = psi % 8
                        psi += 1
                        for kc in range(8):
                            self.mm(self.ps[p][:], hT[:, kc, i * 128:(i + 1) * 128], W[wb][:, kc, :],
                                    kc == 0, kc == 7, [khT[i], kW[wb]], [self.psk[p]])
                        sb = sti % 4
                        sti += 1
                        if grp < 2:
                            ct, sn = (cos_t, sin_t) if grp == 0 else (cosk_t, sink_t)
                            kt_ = ksc if grp == 0 else ksck
                            pv = self.ps[p][:].rearrange("p (h two f) -> p h two f", two=2, f=64)
                            x1 = pv[:, :, 0, :]
                            x2 = pv[:, :, 1, :]
                            cb = ct[:, tile, :].unsqueeze(1).broadcast_to([128, 4, 64])
                            sb_ = sn[:, tile, :].unsqueeze(1).broadcast_to([128, 4, 64])
                            ov = st[sb][:].rearrange("p (h two f) -> p h two f", two=2, f=64)
                            rb = (sti) % 2
                            R = rt[rb]
                            self.dve(lambda e, R=R, x1=x1, cb=cb: e.tensor_tensor(R[:, 0], x1, cb, ALU.mult),
                                     [self.psk[p], kt_], [krt[rb]])
                            self.dve(lambda e, R=R, x2=x2, sb_=sb_: e.tensor_tensor(R[:, 1], x2, sb_, ALU.mult),
                                     [self.psk[p], kt_], [krt[rb]])
                            self.dve(lambda e, R=R, x2=x2, cb=cb: e.tensor_tensor(R[:, 2], x2, cb, ALU.mult),
                                     [self.psk[p], kt_], [krt[rb]])
                            self.dve(lambda e, R=R, x1=x1, sb_=sb_: e.tensor_tensor(R[:, 3], x1, sb_, ALU.mult),
                                     [self.psk[p], kt_], [krt[rb]])
                            self.dve(lambda e, R=R, ov=ov: e.tensor_tensor(ov[:, :, 0, :], R[:, 0], R[:, 1], ALU.subtract),
                                     [krt[rb]], [kst[sb]])
                            self.dve(lambda e, R=R, ov=ov: e.tensor_tensor(ov[:, :, 1, :], R[:, 2], R[:, 3], ALU.add),
                                     [krt[rb]], [kst[sb]])
                        else:
                            self.act(st[sb][:], self.ps[p][:], AF.Copy, [self.psk[p]], [kst[sb]])
                        self.dma(ztm_v[tile][:, grp * 512:(grp + 1) * 512], st[sb][:], [kst[sb]], [])
                else:
                    for sub in range(4):
                        c = (grp - 6) * 4 + sub
                        p = psi % 8
                        psi += 1
                        for kc in range(8):
                            self.mm(self.ps[p][:, 0:BLK], W[wb][:, kc, sub * 128:(sub + 1) * 128], hT[:, kc, :],
                                    kc == 0, kc == 7, khT + [kW[wb]], [self.psk[p]])
                        sb = sti % 4
                        sti += 1
                        self.act(st[sb][:, 0:BLK], self.ps[p][:, 0:BLK], AF.Copy, [self.psk[p]], [kst[sb]])
                        self.dma(zfm[c][:, blk * BLK:(blk + 1) * BLK], st[sb][:, 0:BLK], [kst[sb]], [])
                        if xh_d is not None and blk == 0 and c < 8:
                            p = psi % 8
                            psi += 1
                            for kc in range(8):
                                self.mm(self.ps[p][:, 0:3], W[wb][:, kc, sub * 128:(sub + 1) * 128], hhT[:, kc, 0:3],
                                        kc == 0, kc == 7, [khh, kW[wb]], [self.psk[p]])
                            self.act(sth[:, c, 0:3], self.ps[p][:, 0:3], AF.Copy, [self.psk[p]], [ksth])
            if xh_d is not None and blk == 0:
                self.dma(zh.rearrange("c p t -> p c t"), sth[:, :, 0:3], [ksth], [], slow=True)
        S.barrier()


def build_test_A(T):
    k = K(T, "A", False)
    x = k.inp("x", [T, D]); xh = k.inp("xh", [3, D]); w_in = k.inp("w_in", [D, 7168])
    g = k.inp("g_mix", [D]); pos = k.inp("pos", [T], I32); invf = k.inp("invf", [64])
    ztm = k.outp("ztm", [T, 3072]); zfm = k.outp("zfm", [32, 128, T]); zh = k.outp("zh", [8, 128, 3])
    k.consts()
    k.stage_A(x, xh, w_in, g, pos, invf, ztm, zfm, zh)
    k.S.emit()
    return k


LOG_G = [math.log(1.0 - 2.0 ** (-5.0 - h)) for h in range(4)]


def _ret_tables(self):
    ar = self.ar
    kdec = ar.t([128, 4]); qdT = ar.t([128, 4, 128]); decT = ar.t([128, 4, 128])
    tmpc = ar.t([128, 1]); tmpm = ar.t([128, 128]); tmpr = ar.t([128, 128])
    kt = self.key()
    self.kret = kt
    self.pool(lambda e: e.iota(tmpc[:], pattern=[[0, 1]], base=127, channel_multiplier=-1, allow_small_or_imprecise_dtypes=True), [], [kt])
    self.pool(lambda e: e.iota(tmpm[:], pattern=[[1, 128]], base=0, channel_multiplier=-1, allow_small_or_imprecise_dtypes=True), [], [kt])
    self.pool(lambda e: e.iota(tmpr[:], pattern=[[1, 128]], base=1, channel_multiplier=0, allow_small_or_imprecise_dtypes=True), [], [kt])
    for h in range(4):
        self.act(kdec[:, h:h + 1], tmpc[:], AF.Exp, [kt], [kt], scale=LOG_G[h])
        self.act(qdT[:, h, :], tmpr[:], AF.Exp, [kt], [kt], scale=LOG_G[h])
        self.act(decT[:, h, :], tmpm[:], AF.Exp, [kt], [kt], scale=LOG_G[h])
        self.pool(lambda e, h=h: e.affine_select(decT[:, h, :], decT[:, h, :], pattern=[[1, 128]],
                                                 compare_op=ALU.is_ge, fill=0.0, base=0,
                                                 channel_multiplier=-1), [kt], [kt])
    self.kdec, self.qdT, self.decT = kdec, qdT, decT


def stage_B1(self, ztm, send_out):
    T, NT = self.T, self.NT
    ar = self.ar
    ar.reset()
    _ret_tables(self)
    kt = self.kret
    ztm_v = ztm.rearrange("(n p) c -> n p c", p=128)
    kb = [ar.t([128, 512]) for _ in range(2)]; kkb = [self.key() for _ in range(2)]
    vb = [ar.t([128, 1024]) for _ in range(2)]; kvb = [self.key() for _ in range(2)]
    kd = [ar.t([128, 4, 128]) for _ in range(2)]; kkd = [self.key() for _ in range(2)]
    Sb = [ar.t([128, 4, 256]) for _ in range(2)]; kS = [self.key() for _ in range(2)]
    self.dve(lambda e: e.memset(Sb[0][:], 0.0), [], [kS[0]])
    psi = 0
    for n in range(NT):
        b = n % 2
        self.dma(kb[b][:], ztm_v[n][:, 512:1024], [], [kkb[b]])
        self.dma(vb[b][:], ztm_v[n][:, 1024:2048], [], [kvb[b]])
        self.dve(lambda e, b=b: e.tensor_tensor(kd[b][:], kb[b][:].rearrange("p (h d) -> p h d", h=4),
                                                self.kdec[:].unsqueeze(2).broadcast_to([128, 4, 128]), ALU.mult),
                 [kkb[b], kt], [kkd[b]])
        so, sn = Sb[n % 2], Sb[(n + 1) % 2]
        for hp in range(2):
            p = psi % 8
            psi += 1
            for e2 in range(2):
                h = hp * 2 + e2
                self.mm(self.ps[p][:, e2 * 256:(e2 + 1) * 256], kd[b][:, h, :], vb[b][:, h * 256:(h + 1) * 256],
                        True, True, [kkd[b], kvb[b]], [self.psk[p]])
            for e2 in range(2):
                h = hp * 2 + e2
                cd = math.exp(128 * LOG_G[h])
                self.dve(lambda e, h=h, cd=cd, p=p, e2=e2, so=so, sn=sn: e.scalar_tensor_tensor(
                    sn[:, h, :], so[:, h, :], cd, self.ps[p][:, e2 * 256:(e2 + 1) * 256], ALU.mult, ALU.add),
                    [kS[n % 2], self.psk[p]], [kS[(n + 1) % 2]])
    self.dma(send_out, Sb[NT % 2][:].rearrange("p h e -> p (h e)"), [kS[NT % 2]], [])
    self.S.barrier()


def stage_B2(self, ztm, send_all, csel_d, retT_d):
    T, NT = self.T, self.NT
    ar = self.ar
    ar.reset()
    _ret_tables(self)
    kt = self.kret
    ztm_v = ztm.rearrange("(n p) c -> n p c", p=128)
    csel = ar.t([128, 8]); kcs = self.key()
    self.load_bcast(csel[:], csel_d, kcs)
    fD = ar.t([128, 8, 4]); kfD = self.key()
    for h in range(4):
        Dh = math.exp(T * LOG_G[h])
        self.dve(lambda e, h=h, Dh=Dh: e.tensor_scalar(fD[:, :, h], csel[:], Dh - 1.0, 1.0, ALU.mult, ALU.add),
                 [kcs], [kfD])
    Sb = [ar.t([128, 4, 256]) for _ in range(2)]; kS = [self.key() for _ in range(2)]
    se = [ar.t([128, 1024]) for _ in range(2)]; kse = [self.key() for _ in range(2)]
    self.dve(lambda e: e.memset(Sb[0][:], 0.0), [], [kS[0]])
    for c in range(NCORES):
        b = c % 2
        self.dma(se[b][:], send_all[c], [], [kse[b]])
        self.dve(lambda e, b=b, c=c: e.tensor_scalar(se[b][:], se[b][:], csel[:, c:c + 1], None, ALU.mult),
                 [kse[b], kcs], [kse[b]])
        for h in range(4):
            self.dve(lambda e, b=b, c=c, h=h: e.scalar_tensor_tensor(
                Sb[0][:, h, :], Sb[0][:, h, :], fD[:, c, h:h + 1], se[b][:, h * 256:(h + 1) * 256],
                ALU.mult, ALU.add), [kS[0], kfD, kse[b]], [kS[0]])
    qk = [ar.t([128, 1024]) for _ in range(2)]; kqk = [self.key() for _ in range(2)]
    vb = [ar.t([128, 1024]) for _ in range(2)]; kvb = [self.key() for _ in range(2)]
    gb = [ar.t([128, 1024]) for _ in range(2)]; kgb = [self.key() for _ in range(2)]
    qT = ar.t([128, 4, 128]); kqT = self.key()
    qdT = ar.t([128, 4, 128]); kqd = self.key()
    kT = ar.t([128, 4, 128]); kkT = self.key()
    kd = ar.t([128, 4, 128]); kkd = self.key()
    sT = ar.t([128, 4, 128]); ksT = self.key()
    s12 = ar.t([128, 8]); mv2 = ar.t([128, 8]); rs = ar.t([128, 4]); sq = ar.t([128, 4]); kst = self.key()
    on = ar.t([128, 1024]); kon = self.key()
    sg = ar.t([128, 1024]); ksg = self.key()
    rT = [ar.t([128, 8, 128]) for _ in range(2)]; krT = [self.key() for _ in range(2)]
    retT_v = retT_d.rearrange("c p t -> p c t")
    psi = 0

    def nb():
        nonlocal psi
        p = psi % 8
        psi += 1
        return p

    for n in range(NT):
        b = n % 2
        self.dma(qk[b][:], ztm_v[n][:, 0:1024], [], [kqk[b]])
        self.dma(vb[b][:], ztm_v[n][:, 1024:2048], [], [kvb[b]])
        self.dma(gb[b][:], ztm_v[n][:, 2048:3072], [], [kgb[b]])
        pq, pk = nb(), nb()
        for h in range(4):
            self.tr(self.ps[pq][:, h * 128:(h + 1) * 128], qk[b][:, h * 128:(h + 1) * 128], self.ident[:],
                    [kqk[b], self.kI], [self.psk[pq]])
        for h in range(4):
            self.tr(self.ps[pk][:, h * 128:(h + 1) * 128], qk[b][:, 512 + h * 128:512 + (h + 1) * 128], self.ident[:],
                    [kqk[b], self.kI], [self.psk[pk]])
        self.act(qT[:].rearrange("p h i -> p (h i)"), self.ps[pq][:], AF.Copy, [self.psk[pq]], [kqT])
        self.dve(lambda e: e.tensor_tensor(qdT[:].rearrange("p h i -> p (h i)"), qT[:].rearrange("p h i -> p (h i)"),
                                           self.qdT[:].rearrange("p h i -> p (h i)"), ALU.mult),
                 [kqT, kt], [kqd])
        self.act(kT[:].rearrange("p h i -> p (h i)"), self.ps[pk][:], AF.Copy, [self.psk[pk]], [kkT])
        self.dve(lambda e, b=b: e.tensor_tensor(kd[:], qk[b][:, 512:1024].rearrange("p (h d) -> p h d", h=4),
                                                self.kdec[:].unsqueeze(2).broadcast_to([128, 4, 128]), ALU.mult),
                 [kqk[b], kt], [kkd])
        pc = nb()
        for h in range(4):
            self.mm(self.ps[pc][:, h * 128:(h + 1) * 128], kT[:, h, :], qT[:, h, :], True, True,
                    [kkT, kqT], [self.psk[pc]])
        self.dve(lambda e, pc=pc: e.tensor_tensor(sT[:].rearrange("p h i -> p (h i)"), self.ps[pc][:],
                                                  self.decT[:].rearrange("p h i -> p (h i)"), ALU.mult),
                 [self.psk[pc], kt], [ksT])
        so, sn = Sb[n % 2], Sb[(n + 1) % 2]
        po = [nb(), nb()]
        for h in range(4):
            p = po[h // 2]
            o_ap = self.ps[p][:, (h % 2) * 256:(h % 2 + 1) * 256]
            self.mm(o_ap, sT[:, h, :], vb[b][:, h * 256:(h + 1) * 256], True, False, [ksT, kvb[b]], [self.psk[p]])
            self.mm(o_ap, qdT[:, h, :], so[:, h, :], False, True, [kqd, kS[n % 2]], [self.psk[p]])
        pkv = [nb(), nb()]
        for h in range(4):
            p = pkv[h // 2]
            self.mm(self.ps[p][:, (h % 2) * 256:(h % 2 + 1) * 256], kd[:, h, :], vb[b][:, h * 256:(h + 1) * 256],
                    True, True, [kkd, kvb[b]], [self.psk[p]])
        for h in range(4):
            p = pkv[h // 2]
            cd = math.exp(128 * LOG_G[h])
            self.dve(lambda e, h=h, cd=cd, p=p, so=so, sn=sn: e.scalar_tensor_tensor(
                sn[:, h, :], so[:, h, :], cd, self.ps[p][:, (h % 2) * 256:(h % 2 + 1) * 256], ALU.mult, ALU.add),
                [kS[n % 2], self.psk[p]], [kS[(n + 1) % 2]])
        for h in range(4):
            p = po[h // 2]
            o_ap = self.ps[p][:, (h % 2) * 256:(h % 2 + 1) * 256]
            self.act(sg[:, 0:256], o_ap, AF.Copy, [self.psk[p]], [ksg, kst], accum_out=s12[:, h:h + 1])
            self.act(sg[:, 256:512], o_ap, AF.Square, [self.psk[p]], [ksg, kst], accum_out=s12[:, 4 + h:5 + h])
        self.dve(lambda e: e.tensor_scalar(mv2[:], s12[:], 1.0 / 256.0, None, ALU.mult), [kst], [kst])
        self.dve(lambda e: e.tensor_tensor(sq[:], mv2[:, 0:4], mv2[:, 0:4], ALU.mult), [kst], [kst])
        self.dve(lambda e: e.tensor_tensor(sq[:], mv2[:, 4:8], sq[:], ALU.subtract), [kst], [kst])
        self.dve(lambda e: e.tensor_scalar(sq[:], sq[:], EPS, None, ALU.add), [kst], [kst])
        self.act(sq[:], sq[:], AF.Sqrt, [kst], [kst])
        self.dve(lambda e: e.reciprocal(rs[:], sq[:]), [kst], [kst])
        for h in range(4):
            p = po[h // 2]
            o_ap = self.ps[p][:, (h % 2) * 256:(h % 2 + 1) * 256]
            self.dve(lambda e, h=h, o_ap=o_ap: e.tensor_scalar(on[:, h * 256:(h + 1) * 256], o_ap, mv2[:, h:h + 1],
                                                               rs[:, h:h + 1], ALU.subtract, ALU.mult),
                     [self.psk[p], kst], [kon])
        self.act(sg[:], gb[b][:], AF.Sigmoid, [kgb[b], kst], [ksg])
        self.dve(lambda e, b=b: e.tensor_tensor(sg[:], sg[:], gb[b][:], ALU.mult), [ksg, kgb[b]], [ksg])
        self.dve(lambda e: e.tensor_tensor(on[:], on[:], sg[:], ALU.mult), [kon, ksg], [kon])
        for half in range(2):
            p = nb()
            for j in range(4):
                kc = half * 4 + j
                self.tr(self.ps[p][:, j * 128:(j + 1) * 128], on[:, kc * 128:(kc + 1) * 128], self.ident[:],
                        [kon, self.kI], [self.psk[p]])
            self.act(rT[b][:, half * 4:(half + 1) * 4, :], self.ps[p][:].rearrange("p (j c) -> p j c", c=128),
                     AF.Copy, [self.psk[p]], [krT[b]])
        self.dma(retT_v[:, :, n * 128:(n + 1) * 128], rT[b][:], [krT[b]], [])
    self.S.barrier()


def gelu_tanh(self, out, y, t1, reads, kout, kt1):
    self.dve(lambda e: e.tensor_tensor(t1, y, y, ALU.mult), reads, [kt1])
    self.dve(lambda e: e.tensor_scalar(t1, t1, 0.044715, 1.0, ALU.mult, ALU.add), [kt1], [kt1])
    self.dve(lambda e: e.tensor_tensor(t1, t1, y, ALU.mult), [kt1] + list(reads), [kt1])
    self.act(t1, t1, AF.Sigmoid, [kt1], [kt1], scale=1.5957691216057308)
    self.dve(lambda e: e.tensor_tensor(out, y, t1, ALU.mult), [kt1] + list(reads), [kout])


def stage_C(self, zfm, zh, pos_d, conv_w, conv_b, wa_d, ba_d, wx_d, bx_d, lam_d, mode, lsum_out=None,
            lsum_all=None, csel_d=None, rnnT_d=None):
    T = self.T
    ar = self.ar
    ar.reset()
    kc_ = self.key()
    cw = ar.t([128, 4, 8]); cb_ = ar.t([128, 8]); ba = ar.t([128, 8]); bx = ar.t([128, 8]); lam = ar.t([128, 8])
    for j in range(4):
        self.dma(cw[:, j, :], conv_w[j].rearrange("(cb p) -> p cb", p=128), [], [kc_], slow=True)
    self.dma(cb_[:], conv_b.rearrange("(cb p) -> p cb", p=128), [], [kc_], slow=True)
    self.dma(ba[:], ba_d.rearrange("cb p -> p cb"), [], [kc_], slow=True)
    self.dma(bx[:], bx_d.rearrange("cb p -> p cb"), [], [kc_], slow=True)
    self.dma(lam[:], lam_d.rearrange("(cb p) -> p cb", p=128), [], [kc_], slow=True)
    wa = ar.t([128, 8, 128]); wx = ar.t([128, 8, 128]); kw = self.key()
    self.dma(wa[:], wa_d.rearrange("cb c d -> c cb d"), [], [kw])
    self.dma(wx[:], wx_d.rearrange("cb c d -> c cb d"), [], [kw])
    nsp = ar.t([128, 8]); knsp = self.key()
    self.act(nsp[:], lam[:], AF.Exp, [kc_], [knsp], scale=-1.0)
    self.dve(lambda e: e.tensor_scalar(nsp[:], nsp[:], 1.0, None, ALU.add), [knsp], [knsp])
    self.act(nsp[:], nsp[:], AF.Ln, [knsp], [knsp])
    self.dve(lambda e: e.tensor_scalar(nsp[:], nsp[:], -8.0, None, ALU.mult), [knsp], [knsp])
    posb = ar.t([128, T], I32); kpb = self.key()
    self.dma(posb[:], pos_d.partition_broadcast(128), [], [kpb])
    notm = ar.t([128, T]); m0 = ar.t([128, T]); km = self.key()
    self.dve(lambda e: e.tensor_single_scalar(notm[:], posb[:], 0, ALU.not_equal), [kpb], [km])
    self.dve(lambda e: e.tensor_scalar(m0[:], notm[:], -1.0, 1.0, ALU.mult, ALU.add), [km], [km])
    hin = None
    if mode == "P2":
        csel = ar.t([128, 8]); kcs = self.key()
        self.load_bcast(csel[:], csel_d, kcs)
        la = ar.t([128, 8, 16]); kla = self.key()
        self.dma(la[:], lsum_all.rearrange("c p k -> p c k"), [], [kla])
        hin = ar.t([128, 8]); khin = self.key()
        fP = ar.t([128, 8]); hs = ar.t([128, 8])
        self.dve(lambda e: e.memset(hin[:], 0.0), [], [khin])
        for c in range(NCORES):
            self.dve(lambda e, c=c: e.tensor_scalar(fP[:], la[:, c, 0:8], -1.0, csel[:, c:c + 1], ALU.add, ALU.mult),
                     [kla, kcs], [khin])
            self.dve(lambda e: e.tensor_scalar(fP[:], fP[:], 1.0, None, ALU.add), [khin], [khin])
            self.dve(lambda e, c=c: e.tensor_scalar(hs[:], la[:, c, 8:16], csel[:, c:c + 1], None, ALU.mult),
                     [kla, kcs], [khin])
            self.dve(lambda e: e.tensor_tensor(hin[:], hin[:], fP[:], ALU.mult), [khin], [khin])
            self.dve(lambda e: e.tensor_tensor(hin[:], hin[:], hs[:], ALU.add), [khin], [khin])
    else:
        lsum = ar.t([128, 16]); kls = self.key()
        zeros = ar.t([128, T]); kz = self.key()
        self.dve(lambda e: e.memset(zeros[:], 0.0), [], [kz])
    xin = ar.t([128, T + 4]); kxin = self.key()
    xc = ar.t([128, T]); kxc = self.key()
    gr = ar.t([128, T]); kgr = self.key()
    gi = ar.t([128, T]); kgi = self.key()
    a = ar.t([128, T]); ka = self.key()
    mu = ar.t([128, T]); kmu = self.key()
    bb = ar.t([128, T]); kbb = self.key()
    hh = ar.t([128, T]); khh = self.key()
    if mode == "P2":
        yb = ar.t([128, T]); kyb = self.key()
        t1 = ar.t([128, T]); kt1 = self.key()
    psi = 0
    NTB = T // 512 if T >= 512 else 1
    TB = min(512, T)
    for cb in range(8):
        self.dma(xin[:, 0:3], zh[cb], [], [kxin], slow=True)
        self.dma(xin[:, 3:3 + T], zfm[cb], [], [kxin])
        self.dve(lambda e, cb=cb: e.tensor_scalar(xc[:], xin[:, 0:T], cw[:, 0, cb:cb + 1], cb_[:, cb:cb + 1],
                                                  ALU.mult, ALU.add), [kxin, kc_], [kxc])
        for j in range(1, 4):
            self.dve(lambda e, cb=cb, j=j: e.scalar_tensor_tensor(xc[:], xin[:, j:j + T], cw[:, j, cb:cb + 1], xc[:],
                                                                  ALU.mult, ALU.add), [kxin, kc_, kxc], [kxc])
        for tb in range(NTB):
            for (wt, bt, go, kgo) in ((wa, ba, gr, kgr), (wx, bx, gi, kgi)):
                p = psi % 8
                psi += 1
                self.mm(self.ps[p][:, 0:TB], wt[:, cb, :], xc[:, tb * TB:(tb + 1) * TB], True, True,
                        [kw, kxc], [self.psk[p]])
                self.act(go[:, tb * TB:(tb + 1) * TB], self.ps[p][:, 0:TB], AF.Sigmoid, [self.psk[p], kc_], [kgo],
                         bias=bt[:, cb:cb + 1])
        self.act(a[:], gr[:], AF.Exp, [kgr, knsp], [ka], scale=nsp[:, cb:cb + 1])
        self.dve(lambda e: e.tensor_tensor(mu[:], a[:], a[:], ALU.mult), [ka], [kmu])
        self.dve(lambda e: e.tensor_scalar(mu[:], mu[:], -1.0, 1.0, ALU.mult, ALU.add), [kmu], [kmu])
        self.act(mu[:], mu[:], AF.Sqrt, [kmu], [kmu])
        self.dve(lambda e: e.tensor_tensor(a[:], a[:], notm[:], ALU.mult), [ka, km], [ka])
        self.dve(lambda e: e.tensor_tensor(mu[:], mu[:], notm[:], ALU.mult), [kmu, km], [kmu])
        self.dve(lambda e: e.tensor_tensor(mu[:], mu[:], m0[:], ALU.add), [kmu, km], [kmu])
        self.dve(lambda e: e.tensor_tensor(bb[:], xc[:], gi[:], ALU.mult), [kxc, kgi], [kbb])
        self.dve(lambda e: e.tensor_tensor(bb[:], bb[:], mu[:], ALU.mult), [kbb, kmu], [kbb])
        if mode == "P1":
            self.dve(lambda e: e.tensor_tensor_scan(hh[:], a[:], bb[:], 0.0, ALU.mult, ALU.add), [ka, kbb], [khh])
            self.dve(lambda e, cb=cb: e.tensor_copy(lsum[:, 8 + cb:9 + cb], hh[:, T - 1:T]), [khh], [kls])
            self.dve(lambda e: e.tensor_tensor_scan(hh[:], a[:], zeros[:], 1.0, ALU.mult, ALU.add), [ka, kz], [khh])
            self.dve(lambda e, cb=cb: e.tensor_copy(lsum[:, cb:cb + 1], hh[:, T - 1:T]), [khh], [kls])
        else:
            self.dve(lambda e, cb=cb: e.tensor_tensor_scan(hh[:], a[:], bb[:], hin[:, cb:cb + 1], ALU.mult, ALU.add),
                     [ka, kbb, khin], [khh])
            self.dma(yb[:], zfm[8 + cb], [], [kyb])
            gelu_tanh(self, yb[:], yb[:], t1[:], [kyb], kyb, kt1)
            self.dve(lambda e: e.tensor_tensor(hh[:], hh[:], yb[:], ALU.mult), [khh, kyb], [khh])
            self.dma(rnnT_d[cb], hh[:], [khh], [])
    if mode == "P1":
        self.dma(lsum_out, lsum[:], [kls], [])
    self.S.barrier()


def stage_D(self, retT_d, rnnT_d, zfm, w_ret, w_rnn, w_mix, x_src, x_dst):
    T, BLK = self.T, self.BLK
    NB = BLK // 128
    ar = self.ar
    ar.reset()
    Wr = ar.t([128, 8, 1024]); Wn = ar.t([128, 8, 1024]); Wm = ar.t([128, 8, 1024]); kW = self.key()
    self.dma(Wr[:], w_ret.rearrange("(kc p) c -> p kc c", p=128), [], [kW])
    self.dma(Wn[:], w_rnn.rearrange("(kc p) c -> p kc c", p=128), [], [kW])
    self.dma(Wm[:], w_mix.rearrange("(kc p) c -> p kc c", p=128), [], [kW])
    rT = ar.t([128, 8, BLK]); nT = ar.t([128, 8, BLK]); kin = self.key()
    mT = ar.t([128, 8, BLK]); kmT = self.key()
    ga = [ar.t([128, BLK]) for _ in range(2)]; gb = [ar.t([128, BLK]) for _ in range(2)]
    kga = [self.key() for _ in range(2)]
    m1 = [ar.t([128, BLK]) for _ in range(2)]; km1 = [self.key() for _ in range(2)]
    xt = [ar.t([128, D]) for _ in range(2)]; kx = [self.key() for _ in range(2)]
    retT_v = retT_d.rearrange("c p t -> p c t")
    rnnT_v = rnnT_d.rearrange("c p t -> p c t")
    x_sv = x_src.rearrange("(n p) c -> n p c", p=128)
    x_dv = x_dst.rearrange("(n p) c -> n p c", p=128)
    psi = 0
    for blk in range(T // BLK):
        sl = slice(blk * BLK, (blk + 1) * BLK)
        self.dma(rT[:], retT_v[:, :, sl], [], [kin])
        self.dma(nT[:], rnnT_v[:, :, sl], [], [kin])
        for dc in range(8):
            b = dc % 2
            self.dma(ga[b][:], zfm[16 + dc][:, sl], [], [kga[b]])
            self.dma(gb[b][:], zfm[24 + dc][:, sl], [], [kga[b]])
            self.act(ga[b][:], ga[b][:], AF.Sigmoid, [kga[b]], [kga[b]])
            self.act(gb[b][:], gb[b][:], AF.Sigmoid, [kga[b]], [kga[b]])
            pr = psi % 8; psi += 1
            pn = psi % 8; psi += 1
            for kc in range(8):
                self.mm(self.ps[pr][:, 0:BLK], Wr[:, kc, dc * 128:(dc + 1) * 128], rT[:, kc, :], kc == 0, kc == 7,
                        [kW, kin], [self.psk[pr]])
            for kc in range(8):
                self.mm(self.ps[pn][:, 0:BLK], Wn[:, kc, dc * 128:(dc + 1) * 128], nT[:, kc, :], kc == 0, kc == 7,
                        [kW, kin], [self.psk[pn]])
            self.dve(lambda e, b=b, pr=pr: e.tensor_tensor(m1[b][:], self.ps[pr][:, 0:BLK], ga[b][:], ALU.mult),
                     [self.psk[pr], kga[b]], [km1[b]])
            self.dve(lambda e, b=b, pn=pn: e.tensor_tensor(gb[b][:], self.ps[pn][:, 0:BLK], gb[b][:], ALU.mult),
                     [self.psk[pn], kga[b]], [kga[b]])
            self.dve(lambda e, b=b, dc=dc: e.tensor_tensor(mT[:, dc, :], m1[b][:], gb[b][:], ALU.add),
                     [km1[b], kga[b]], [kmT])
        for i in range(NB):
            tile = blk * NB + i
            b = tile % 2
            self.dma(xt[b][:], x_sv[tile], [], [kx[b]])
            for half in range(2):
                p = psi % 8; psi += 1
                for kc in range(8):
                    self.mm(self.ps[p][:], mT[:, kc, i * 128:(i + 1) * 128], Wm[:, kc, half * 512:(half + 1) * 512],
                            kc == 0, kc == 7, [kW, kmT], [self.psk[p]])
                self.dve(lambda e, b=b, p=p, half=half: e.tensor_tensor(
                    xt[b][:, half * 512:(half + 1) * 512], xt[b][:, half * 512:(half + 1) * 512], self.ps[p][:],
                    ALU.add), [kx[b], self.psk[p]], [kx[b]])
            self.dma(x_dv[tile], xt[b][:], [kx[b]], [])
    self.S.barrier()


K.stage_B1 = stage_B1
K.stage_B2 = stage_B2
K.stage_C = stage_C
K.stage_D = stage_D


def stage_E0(self, mem_d, g_mem, w_xk, w_xv, kT_d, v_d):
    ar = self.ar
    ar.reset()
    gbm = ar.t([128, D]); kg = self.key()
    self.load_bcast(gbm[:], g_mem, kg)
    Wk = ar.t([128, 8, 1024]); Wv = ar.t([128, 8, 1024]); kW = self.key()
    self.dma(Wk[:], w_xk.rearrange("(kc p) c -> p kc c", p=128), [], [kW])
    self.dma(Wv[:], w_xv.rearrange("(kc p) c -> p kc c", p=128), [], [kW])
    mT = ar.t([128, 8, 256]); kmT = self.key()
    xt = ar.t([128, D]); kx = self.key()
    hb = ar.t([128, D]); kh = self.key()
    junk = ar.t([128, D]); kj = self.key()
    ss = ar.t([128, 4]); kss = self.key()
    st = [ar.t([128, 512]) for _ in range(2)]; kst = [self.key() for _ in range(2)]
    mem_v = mem_d.rearrange("(n p) c -> n p c", p=128)
    v_v = v_d.rearrange("(n p) c -> n p c", p=128)
    psi = 0
    for mt in range(2):
        self.dma(xt[:], mem_v[mt], [], [kx])
        self.rmsnorm_rows(xt, kx, gbm, kg, hb, kh, ss, kss, junk, kj)
        for half in range(2):
            p = psi % 8; psi += 1
            for j in range(4):
                kc = half * 4 + j
                self.tr(self.ps[p][:, j * 128:(j + 1) * 128], hb[:, kc * 128:(kc + 1) * 128], self.ident[:],
                        [kh, self.kI], [self.psk[p]])
            self.act(mT[:, half * 4:(half + 1) * 4, mt * 128:(mt + 1) * 128],
                     self.ps[p][:].rearrange("p (j c) -> p j c", c=128), AF.Copy, [self.psk[p]], [kmT])
    si = 0
    for dc in range(8):
        p = psi % 8; psi += 1
        for kc in range(8):
            self.mm(self.ps[p][:, 0:256], Wk[:, kc, dc * 128:(dc + 1) * 128], mT[:, kc, :], kc == 0, kc == 7,
                    [kW, kmT], [self.psk[p]])
        b = si % 2; si += 1
        self.act(st[b][:, 0:256], self.ps[p][:, 0:256], AF.Copy, [self.psk[p]], [kst[b]])
        self.dma(kT_d[dc], st[b][:, 0:256], [kst[b]], [])
    for mt in range(2):
        for half in range(2):
            p = psi % 8; psi += 1
            for kc in range(8):
                self.mm(self.ps[p][:], mT[:, kc, mt * 128:(mt + 1) * 128], Wv[:, kc, half * 512:(half + 1) * 512],
                        kc == 0, kc == 7, [kW, kmT], [self.psk[p]])
            b = si % 2; si += 1
            self.act(st[b][:], self.ps[p][:], AF.Copy, [self.psk[p]], [kst[b]])
            self.dma(v_v[mt][:, half * 512:(half + 1) * 512], st[b][:], [kst[b]], [])
    self.S.barrier()


def stage_E(self, x_src, x_dst, g_x, w_xq, w_xo, kT_d, v_d):
    T, BLK = self.T, self.BLK
    NB = BLK // 128
    ar = self.ar
    ar.reset()
    gbx = ar.t([128, D]); kg = self.key()
    self.load_bcast(gbx[:], g_x, kg)
    Wq = ar.t([128, 8, 1024]); Wo = ar.t([128, 8, 1024]); kW = self.key()
    self.dma(Wq[:], w_xq.rearrange("(kc p) c -> p kc c", p=128), [], [kW])
    self.dma(Wo[:], w_xo.rearrange("(kc p) c -> p kc c", p=128), [], [kW])
    kT = ar.t([128, 8, 256]); vv = ar.t([128, 2, 1024]); kkv = self.key()
    self.dma(kT[:], kT_d.rearrange("c p m -> p c m"), [], [kkv])
    self.dma(vv[:], v_d.rearrange("(n p) c -> p n c", p=128), [], [kkv])
    xt = [ar.t([128, D]) for _ in range(NB)]; kx = [self.key() for _ in range(NB)]
    hb = ar.t([128, D]); kh = self.key()
    junk = ar.t([128, D]); kj = self.key()
    ss = ar.t([128, 4]); kss = self.key()
    h2T = ar.t([128, 8, BLK]); kh2 = self.key()
    qT = ar.t([128, 8, BLK]); kqT = self.key()
    oT = ar.t([128, 8, BLK]); koT = self.key()
    pb = [ar.t([128, 256]) for _ in range(2)]; kpb = [self.key() for _ in range(2)]
    pT = [ar.t([128, 2, 128]) for _ in range(2)]; kpT = [self.key() for _ in range(2)]
    sm = ar.t([128, 16]); ksm = self.key()
    x_sv = x_src.rearrange("(n p) c -> n p c", p=128)
    x_dv = x_dst.rearrange("(n p) c -> n p c", p=128)
    psi = 0
    hi = 0

    def nb():
        nonlocal psi
        p = psi % 8
        psi += 1
        return p

    for blk in range(T // BLK):
        for i in range(NB):
            tile = blk * NB + i
            self.dma(xt[i][:], x_sv[tile], [], [kx[i]])
            self.rmsnorm_rows(xt[i], kx[i], gbx, kg, hb, kh, ss, kss, junk, kj)
            for half in range(2):
                p = nb()
                for j in range(4):
                    kc = half * 4 + j
                    self.tr(self.ps[p][:, j * 128:(j + 1) * 128], hb[:, kc * 128:(kc + 1) * 128], self.ident[:],
                            [kh, self.kI], [self.psk[p]])
                self.act(h2T[:, half * 4:(half + 1) * 4, i * 128:(i + 1) * 128],
                         self.ps[p][:].rearrange("p (j c) -> p j c", c=128), AF.Copy, [self.psk[p]], [kh2])
        for dc in range(8):
            p = nb()
            for kc in range(8):
                self.mm(self.ps[p][:, 0:BLK], Wq[:, kc, dc * 128:(dc + 1) * 128], h2T[:, kc, :], kc == 0, kc == 7,
                        [kW, kh2], [self.psk[p]])
            self.act(qT[:, dc, :], self.ps[p][:, 0:BLK], AF.Copy, [self.psk[p]], [kqT], scale=1.0 / 16.0)
        for i in range(NB):
            cs = slice(i * 128, (i + 1) * 128)
            for h in range(4):
                p = nb()
                sc = self.ps[p][:, 0:256]
                self.mm(sc, qT[:, 2 * h, cs], kT[:, 2 * h, :], True, False, [kqT, kkv], [self.psk[p]])
                self.mm(sc, qT[:, 2 * h + 1, cs], kT[:, 2 * h + 1, :], False, True, [kqT, kkv], [self.psk[p]])
                b = hi % 2
                hi += 1
                self.dve(lambda e, sc=sc, h=h: e.tensor_reduce(sm[:, h:h + 1], sc, axis=AX.X, op=ALU.max),
                         [self.psk[p]], [ksm])
                self.dve(lambda e, h=h: e.tensor_scalar(sm[:, 4 + h:5 + h], sm[:, h:h + 1], -1.0, None, ALU.mult),
                         [ksm], [ksm])
                self.act(pb[b][:], sc, AF.Exp, [self.psk[p], ksm], [kpb[b], ksm], bias=sm[:, 4 + h:5 + h],
                         accum_out=sm[:, 8 + h:9 + h])
                self.dve(lambda e, h=h: e.reciprocal(sm[:, 12 + h:13 + h], sm[:, 8 + h:9 + h]), [ksm], [ksm])
                self.dve(lambda e, h=h, b=b: e.tensor_scalar(pb[b][:], pb[b][:], sm[:, 12 + h:13 + h], None, ALU.mult),
                         [ksm, kpb[b]], [kpb[b]])
                p2 = nb()
                for mt in range(2):
                    self.tr(self.ps[p2][:, mt * 128:(mt + 1) * 128], pb[b][:, mt * 128:(mt + 1) * 128], self.ident[:],
                            [kpb[b], self.kI], [self.psk[p2]])
                self.act(pT[b][:].rearrange("p m t -> p (m t)"), self.ps[p2][:, 0:256], AF.Copy, [self.psk[p2]], [kpT[b]])
                p3 = nb()
                for e2 in range(2):
                    dc = 2 * h + e2
                    for mt in range(2):
                        self.mm(self.ps[p3][:, e2 * 128:(e2 + 1) * 128], vv[:, mt, dc * 128:(dc + 1) * 128], pT[b][:, mt, :],
                                mt == 0, mt == 1, [kkv, kpT[b]], [self.psk[p3]])
                self.act(oT[:, 2 * h:2 * h + 2, cs], self.ps[p3][:, 0:256].rearrange("p (e t) -> p e t", e=2), AF.Copy,
                         [self.psk[p3]], [koT])
        for i in range(NB):
            tile = blk * NB + i
            cs = slice(i * 128, (i + 1) * 128)
            for half in range(2):
                p = nb()
                for kc in range(8):
                    self.mm(self.ps[p][:], oT[:, kc, cs], Wo[:, kc, half * 512:(half + 1) * 512], kc == 0, kc == 7,
                            [kW, koT], [self.psk[p]])
                self.dve(lambda e, i=i, p=p, half=half: e.tensor_tensor(
                    xt[i][:, half * 512:(half + 1) * 512], xt[i][:, half * 512:(half + 1) * 512], self.ps[p][:],
                    ALU.add), [kx[i], self.psk[p]], [kx[i]])
            self.dma(x_dv[tile], xt[i][:], [kx[i]], [])
    self.S.barrier()


def top16(self, vals, idx, src, tmp, reads, kv, ktmp):
    self.dve(lambda e: e.max(vals[:, 0:8], src), reads, [kv])
    self.dve(lambda e: e.max_index(idx[:, 0:8], vals[:, 0:8], src), list(reads) + [kv], [kv])
    self.dve(lambda e: e.match_replace(tmp, vals[:, 0:8], src, -1e30), list(reads) + [kv], [ktmp])
    self.dve(lambda e: e.max(vals[:, 8:16], tmp), [ktmp], [kv])
    self.dve(lambda e: e.max_index(idx[:, 8:16], vals[:, 8:16], tmp), [ktmp, kv], [kv])


def stage_F(self, x_src, x_dst, g_ffn, w_pq, sk1_d, sk2_d, pu, pv, final, g_final=None, outn=None):
    T, NT = self.T, self.NT
    BLK = min(256, T)
    NB = BLK // 128
    ar = self.ar
    ar.reset()
    gbf = ar.t([128, D]); kg = self.key()
    self.load_bcast(gbf[:], g_ffn, kg)
    if final:
        gfin = ar.t([128, D]); kgf = self.key()
        self.load_bcast(gfin[:], g_final, kgf)
    Wp = ar.t([128, 8, 2048]); kW = self.key()
    self.dma(Wp[:, :, 0:1024], w_pq.rearrange("(kc p) c -> p kc c", p=128)[:, :, 0:1024], [], [kW])
    self.dma(Wp[:, :, 1024:2048], w_pq.rearrange("(kc p) c -> p kc c", p=128)[:, :, 1024:2048], [], [kW])
    skT = ar.t([128, 16, 128]); kskT = self.key()
    NG = 8
    gbuf = [ar.t([128, D]) for _ in range(NG)]; kgb = [self.key() for _ in range(NG)]
    skl = [gbuf[j][:].rearrange("p (h d) -> p h d", h=8) for j in range(2)]; kskl = self.key()
    self.dma(skl[0], sk1_d.rearrange("h n d -> n h d"), [], [kgb[0]])
    self.dma(skl[1], sk2_d.rearrange("h n d -> n h d"), [], [kgb[1]])
    psi = 0

    def nb():
        nonlocal psi
        p = psi % 8
        psi += 1
        return p

    for half in range(2):
        for hg in range(2):
            p = nb()
            for j in range(4):
                h = hg * 4 + j
                self.tr(self.ps[p][:, j * 128:(j + 1) * 128], skl[half][:, h, :], self.ident[:], [kgb[half], self.kI],
                        [self.psk[p]])
            for j in range(4):
                h = hg * 4 + j
                self.act(skT[:, 2 * h + half, :], self.ps[p][:, j * 128:(j + 1) * 128], AF.Copy, [self.psk[p]], [kskT])
    iota16 = ar.t([128, 16]); kio = self.key()
    self.pool(lambda e: e.iota(iota16[:], pattern=[[1, 16]], base=0, channel_multiplier=0, allow_small_or_imprecise_dtypes=True), [], [kio])
    xt = [ar.t([128, D]) for _ in range(NB)]; kx = [self.key() for _ in range(NB)]
    h3 = [ar.t([128, D]) for _ in range(NB)]; kh3 = [self.key() for _ in range(NB)]
    junk = ar.t([128, D]); kj = self.key()
    ss = ar.t([128, 4]); kss = self.key()
    h3T = ar.t([128, 8, BLK]); kh3T = self.key()
    qT = ar.t([128, 16, BLK]); kqT = self.key()
    s_all = ar.t([128, 16, 128]); ksa = self.key()
    tmpN = ar.t([128, 256]); ktmp = self.key()
    v12 = ar.t([128, 16, 16]); i12 = ar.t([128, 16, 16], U32); kv12 = self.key()
    i12f = ar.t([128, 16, 16]); ki12f = self.key()
    cand = ar.t([128, 8, 256]); kcand = self.key()
    cv = ar.t([128, 8, 16]); ci = ar.t([128, 8, 16], U32); kcv = self.key()
    ai = ar.t([128, 8, 16], I32); bi = ar.t([128, 8, 16], I32); af = ar.t([128, 8, 16]); bf = ar.t([128, 8, 16])
    kab = self.key()
    oh = ar.t([128, 8, 16, 16]); koh = self.key()
    e1 = ar.t([128, 8, 16]); e2 = ar.t([128, 8, 16]); ke = self.key()
    idxf = ar.t([128, 128]); idxi = ar.t([128, 128], I32); kidx = self.key()
    gates = ar.t([128, 8, 16]); gs = ar.t([128, 8]); kgt = self.key()
    actv = ar.t([128, 128]); kact = self.key()
    wgt = ar.t([128, 128]); t1 = ar.t([128, 128]); kwg = self.key(); kt1 = self.key()
    gi = 0
    if final:
        ss2 = ar.t([128, 4]); kss2 = self.key()
        ob = ar.t([128, D]); kob = self.key()
    x_sv = x_src.rearrange("(n p) c -> n p c", p=128)
    x_dv = x_dst.rearrange("(n p) c -> n p c", p=128)
    if final:
        outn_v = outn.rearrange("(n p) c -> n p c", p=128)

    def gather(tab, hk):
        nonlocal gi
        g = gi % NG
        gi += 1
        off = bass.IndirectOffsetOnAxis(ap=idxi[:, hk:hk + 1], axis=0)
        def f(e, g=g, off=off):
            if "r" not in self.regc:
                self.regc["r"] = e.to_reg(16383)
            tab_, eo = tab if isinstance(tab, tuple) else (tab, 0)
            return e.indirect_dma_start(out=gbuf[g][:], out_offset=None, in_=tab_, in_offset=off,
                                        element_offset=eo, bounds_check=self.regc["r"], oob_is_err=False)
        self.S.op("pool", f, [kidx], [kgb[g]], dma=True)
        return g

    for blk in range(T // BLK):
        for i in range(NB):
            tile = blk * NB + i
            self.dma(xt[i][:], x_sv[tile], [], [kx[i]])
            self.rmsnorm_rows(xt[i], kx[i], gbf, kg, h3[i], kh3[i], ss, kss, junk, kj)
            for half in range(2):
                p = nb()
                for j in range(4):
                    kc = half * 4 + j
                    self.tr(self.ps[p][:, j * 128:(j + 1) * 128], h3[i][:, kc * 128:(kc + 1) * 128], self.ident[:],
                            [kh3[i], self.kI], [self.psk[p]])
                self.act(h3T[:, half * 4:(half + 1) * 4, i * 128:(i + 1) * 128],
                         self.ps[p][:].rearrange("p (j c) -> p j c", c=128), AF.Copy, [self.psk[p]], [kh3T])
        for c in range(16):
            p = nb()
            for kc in range(8):
                self.mm(self.ps[p][:, 0:BLK], Wp[:, kc, c * 128:(c + 1) * 128], h3T[:, kc, :], kc == 0, kc == 7,
                        [kW, kh3T], [self.psk[p]])
            self.act(qT[:, c, :], self.ps[p][:, 0:BLK], AF.Copy, [self.psk[p]], [kqT])
        for i in range(NB):
            tile = blk * NB + i
            cs = slice(i * 128, (i + 1) * 128)
            for jg in range(4):
                p = nb()
                for jj in range(4):
                    j = jg * 4 + jj
                    self.mm(self.ps[p][:, jj * 128:(jj + 1) * 128], qT[:, j, cs], skT[:, j, :], True, True,
                            [kqT, kskT], [self.psk[p]])
                self.act(s_all[:, jg * 4:(jg + 1) * 4, :], self.ps[p][:].rearrange("p (j c) -> p j c", c=128),
                         AF.Copy, [self.psk[p]], [ksa])
            for j in range(16):
                top16(self, v12[:, j, :], i12[:, j, :], s_all[:, j, :], tmpN[:, 0:128], [ksa], kv12, ktmp)
            self.dve(lambda e: e.tensor_copy(i12f[:], i12[:]), [kv12], [ki12f])
            v12v = v12[:].rearrange("p (h two) k -> p h two k", two=2)
            i12v = i12f[:].rearrange("p (h two) k -> p h two k", two=2)
            self.dve(lambda e, v12v=v12v: e.tensor_tensor(
                cand[:].rearrange("p h (a b) -> p h a b", b=16),
                v12v[:, :, 0, :].unsqueeze(3).broadcast_to([128, 8, 16, 16]),
                v12v[:, :, 1, :].unsqueeze(2).broadcast_to([128, 8, 16, 16]), ALU.add), [kv12], [kcand])
            for h in range(8):
                top16(self, cv[:, h, :], ci[:, h, :], cand[:, h, :], tmpN[:, 0:256], [kcand], kcv, ktmp)
            cii = ci[:].bitcast(I32)
            self.dve(lambda e, cii=cii: e.tensor_single_scalar(ai[:], cii, 4, ALU.logical_shift_right), [kcv], [kab])
            self.dve(lambda e, cii=cii: e.tensor_single_scalar(bi[:], cii, 15, ALU.bitwise_and), [kcv], [kab])
            self.dve(lambda e: e.tensor_copy(af[:], ai[:]), [kab], [kab])
            self.dve(lambda e: e.tensor_copy(bf[:], bi[:]), [kab], [kab])
            io_b = iota16[:].unsqueeze(1).unsqueeze(1).broadcast_to([128, 8, 16, 16])
            for (sel, tabv, eo) in ((af, 0, e1), (bf, 1, e2)):
                self.dve(lambda e, sel=sel, io_b=io_b: e.tensor_tensor(
                    oh[:], sel[:].unsqueeze(3).broadcast_to([128, 8, 16, 16]), io_b, ALU.is_equal),
                    [kab, kio], [koh])
                self.dve(lambda e, tabv=tabv, i12v=i12v: e.tensor_tensor(
                    oh[:], oh[:], i12v[:, :, tabv, :].unsqueeze(2).broadcast_to([128, 8, 16, 16]), ALU.mult),
                    [koh, ki12f], [koh])
                self.dve(lambda e, eo=eo: e.tensor_reduce(eo[:], oh[:], axis=AX.X, op=ALU.add), [koh], [ke])
            self.dve(lambda e: e.scalar_tensor_tensor(idxf[:], e1[:].rearrange("p h k -> p (h k)"), 128.0,
                                                      e2[:].rearrange("p h k -> p (h k)"), ALU.mult, ALU.add),
                     [ke], [kidx])
            self.dve(lambda e: e.tensor_scalar(idxf[:], idxf[:], 0.0, 16383.0, ALU.max, ALU.min), [kidx], [kidx])
            self.dve(lambda e: e.tensor_copy(idxi[:], idxf[:]), [kidx], [kidx])
            self.dve(lambda e: e.tensor_tensor(gates[:], cv[:], cv[:, :, 0:1].broadcast_to([128, 8, 16]), ALU.subtract),
                     [kcv], [kgt])
            self.act(gates[:], gates[:], AF.Exp, [kgt], [kgt])
            self.dve(lambda e: e.tensor_reduce(gs[:], gates[:], axis=AX.X, op=ALU.add), [kgt], [kgt])
            self.dve(lambda e: e.reciprocal(gs[:], gs[:]), [kgt], [kgt])
            self.dve(lambda e: e.tensor_tensor(gates[:], gates[:], gs[:].unsqueeze(2).broadcast_to([128, 8, 16]),
                                               ALU.mult), [kgt], [kgt])
            LOOK = NG - 2
            pend = []
            for hk in range(min(LOOK, 128)):
                pend.append((gather(pu, hk), hk))
            nxt = len(pend)
            for hk in range(128):
                g, hk_ = pend.pop(0)
                self.dve(lambda e, g=g, hk=hk, i=i: e.scalar_tensor_tensor(
                    junk[:], gbuf[g][:], 1.0, h3[i][:], ALU.mult, ALU.mult, accum_out=actv[:, hk:hk + 1]),
                    [kgb[g], kh3[i]], [kj, kact, kgb[g]])
                if nxt < 128:
                    pend.append((gather(pu, nxt), nxt))
                    nxt += 1
            gelu_tanh(self, wgt[:], actv[:], t1[:], [kact], kwg, kt1)
            self.dve(lambda e: e.tensor_tensor(wgt[:], wgt[:], gates[:].rearrange("p h k -> p (h k)"), ALU.mult),
                     [kwg, kgt], [kwg])
            pend = []
            for hk in range(min(LOOK, 128)):
                pend.append((gather(pv, hk), hk))
            nxt = len(pend)
            for hk in range(128):
                g, hk_ = pend.pop(0)
                self.dve(lambda e, g=g, hk=hk, i=i: e.scalar_tensor_tensor(
                    xt[i][:], gbuf[g][:], wgt[:, hk:hk + 1], xt[i][:], ALU.mult, ALU.add),
                    [kgb[g], kwg, kx[i]], [kx[i], kgb[g]])
                if nxt < 128:
                    pend.append((gather(pv, nxt), nxt))
                    nxt += 1
            self.dma(x_dv[tile], xt[i][:], [kx[i]], [])
            if final:
                self.rmsnorm_rows(xt[i], kx[i], gfin, kgf, ob, kob, ss2, kss2, junk, kj)
                self.dma(outn_v[tile], ob[:], [kob], [])
    self.S.barrier()


K.stage_E0 = stage_E0
K.stage_E = stage_E
K.stage_F = stage_F


def build_P1(T):
    k = K(T, "P1", False)
    x = k.inp("x", [T, D]); xh = k.inp("xh", [3, D]); w_in = k.inp("w_in", [D, 7168])
    g = k.inp("g_mix", [D]); pos = k.inp("pos", [T], I32); invf = k.inp("invf", [64])
    conv_w = k.inp("conv_w", [4, D]); conv_b = k.inp("conv_b", [D])
    wa = k.inp("lru_wa", [8, 128, 128]); ba = k.inp("lru_ba", [8, 128])
    wx = k.inp("lru_wx", [8, 128, 128]); bx = k.inp("lru_bx", [8, 128]); lam = k.inp("lru_lam", [D])
    send = k.outp("send", [128, 1024]); lsum = k.outp("lsum", [128, 16])
    ztm = k.scratch("ztm", [T, 3072]); zfm = k.scratch("zfm", [32, 128, T]); zh = k.scratch("zh", [8, 128, 3])
    k.consts()
    k.stage_A(x, xh, w_in, g, pos, invf, ztm, zfm, zh)
    k.stage_B1(ztm, send)
    k.stage_C(zfm, zh, pos, conv_w, conv_b, wa, ba, wx, bx, lam, "P1", lsum_out=lsum)
    k.S.emit()
    return k


def build_P2(T, final, dbg=False, stages="ABCDEF"):
    k = K(T, "P2", final)
    x = k.inp("x", [T, D]); xh = k.inp("xh", [3, D]); w_in = k.inp("w_in", [D, 7168])
    g = k.inp("g_mix", [D]); pos = k.inp("pos", [T], I32); invf = k.inp("invf", [64])
    conv_w = k.inp("conv_w", [4, D]); conv_b = k.inp("conv_b", [D])
    wa = k.inp("lru_wa", [8, 128, 128]); ba = k.inp("lru_ba", [8, 128])
    wx = k.inp("lru_wx", [8, 128, 128]); bx = k.inp("lru_bx", [8, 128]); lam = k.inp("lru_lam", [D])
    send_all = k.inp("send_all", [8, 128, 1024]); lsum_all = k.inp("lsum_all", [8, 128, 16]); csel = k.inp("csel", [8])
    w_ret = k.inp("w_ret_br", [D, D]); w_rnn = k.inp("w_rnn_br", [D, D]); w_mix = k.inp("w_mix_out", [D, D])
    mem = k.inp("mem", [256, D]); g_x = k.inp("g_x", [D]); g_mem = k.inp("g_mem", [D])
    w_xq = k.inp("w_xq", [D, D]); w_xk = k.inp("w_xk", [D, D]); w_xv = k.inp("w_xv", [D, D]); w_xo = k.inp("w_xo", [D, D])
    g_ffn = k.inp("g_ffn", [D]); w_pq = k.inp("w_pq", [D, 2048])
    sk1 = k.inp("sub_k1", [8, 128, 128]); sk2 = k.inp("sub_k2", [8, 128, 128])
    pu = k.inp("peer_u", [16384, D]); pv = k.inp("peer_v", [16384, D]); g_final = k.inp("g_final", [D])
    out = k.outp("out", [T, D]); outn = k.outp("outn", [T, D])
    mk = k.outp if dbg else k.scratch
    ztm = mk("ztm", [T, 3072]); zfm = mk("zfm", [32, 128, T]); zh = mk("zh", [8, 128, 3])
    retT = mk("retT", [8, 128, T]); rnnT = mk("rnnT", [8, 128, T])
    x1 = mk("x1", [T, D]); x2 = mk("x2", [T, D])
    kT = mk("kT", [8, 128, 256]); vm = mk("vm", [256, D])
    k.consts()
    k.stage_A(x, xh, w_in, g, pos, invf, ztm, zfm, zh)
    if "B" in stages:
        k.stage_B2(ztm, send_all, csel, retT)
    if "C" in stages:
        k.stage_C(zfm, zh, pos, conv_w, conv_b, wa, ba, wx, bx, lam, "P2", lsum_all=lsum_all, csel_d=csel, rnnT_d=rnnT)
    if "D" in stages:
        k.stage_D(retT, rnnT, zfm, w_ret, w_rnn, w_mix, x, x1)
    if "E" in stages:
        k.stage_E0(mem, g_mem, w_xk, w_xv, kT, vm)
        k.stage_E(x1, x2, g_x, w_xq, w_xo, kT, vm)
    if "F" in stages:
        k.stage_F(x2 if "E" in stages else x, out, g_ffn, w_pq, sk1, sk2, pu, pv, True, g_final, outn)
    k.S.emit()
    return k


def stage_X(self, parts, oh_d, xin_d, xout_d, W):
    ar = self.ar
    ar.reset()
    ohb = ar.t([128, 8]); koh = self.key()
    self.load_bcast(ohb[:], oh_d, koh)
    summ = ar.t([128, W]); ks = self.key()
    self.dve(lambda e: e.memset(summ[:], 0.0), [], [ks])
    for ap_, off, w in parts:
        self.dma(summ[:, off:off + w], ap_, [], [ks], slow=True)
    xs = ar.t([128, 8, W]); kxs = self.key()
    for c in range(NCORES):
        self.dve(lambda e, c=c: e.tensor_scalar(xs[:, c, :], summ[:], ohb[:, c:c + 1], None, ALU.mult),
                 [ks, koh], [kxs])
    kxi = self.key(); kxo = self.key()
    self.dma(xin_d.rearrange("(c p) w -> p c w", p=128), xs[:], [kxs], [kxi])
    self.S.op("pool", lambda e: e.collective_compute("AllReduce", ALU.add, replica_groups=[list(range(NCORES))],
                                                     ins=[xin_d.opt()], outs=[xout_d.opt()]),
              [kxi], [kxo], dma="cc")
    self.S.barrier()


def stage_H(self, xout_d, ohp_d, zh, W):
    ar = self.ar
    ar.reset()
    ohb = ar.t([128, 8]); koh = self.key()
    self.load_bcast(ohb[:], ohp_d, koh)
    tl = ar.t([128, 8, 24]); kt = self.key()
    self.dma(tl[:], xout_d.rearrange("(c p) w -> p c w", p=128)[:, :, 0:24], [], [kt], slow=True)
    acc = ar.t([128, 24]); ka = self.key()
    self.dve(lambda e: e.memset(acc[:], 0.0), [], [ka])
    for c in range(NCORES):
        self.dve(lambda e, c=c: e.scalar_tensor_tensor(acc[:], tl[:, c, :], ohb[:, c:c + 1], acc[:], ALU.mult, ALU.add),
                 [kt, koh, ka], [ka])
    self.dma(zh.rearrange("c p t -> p c t"), acc[:].rearrange("p (c t) -> p c t", t=3), [ka], [], slow=True)
    self.S.barrier()


K.stage_X = stage_X
K.stage_H = stage_H


def build_fused(T, L=DEPTH):
    k = K(T, "F", True)
    x = k.inp("x", [T, D]); xh = k.inp("xh", [3, D]); pos = k.inp("pos", [T], I32); invf = k.inp("invf", [64])
    csel = k.inp("csel", [8]); oh = k.inp("oh", [8]); ohp = k.inp("ohp", [8])
    mem = k.inp("mem", [256, D]); g_final = k.inp("g_final", [D])
    w_in = k.inp("w_in", [L, D, 7168]); g_mix = k.inp("g_mix", [L, D])
    conv_w = k.inp("conv_w", [L, 4, D]); conv_b = k.inp("conv_b", [L, D])
    wa = k.inp("lru_wa", [L, 8, 128, 128]); ba = k.inp("lru_ba", [L, 8, 128])
    wx = k.inp("lru_wx", [L, 8, 128, 128]); bx = k.inp("lru_bx", [L, 8, 128]); lam = k.inp("lru_lam", [L, D])
    w_ret = k.inp("w_ret_br", [L, D, D]); w_rnn = k.inp("w_rnn_br", [L, D, D]); w_mix = k.inp("w_mix_out", [L, D, D])
    g_x = k.inp("g_x", [L, D]); g_mem = k.inp("g_mem", [L, D])
    w_xq = k.inp("w_xq", [L, D, D]); w_xk = k.inp("w_xk", [L, D, D]); w_xv = k.inp("w_xv", [L, D, D])
    w_xo = k.inp("w_xo", [L, D, D])
    g_ffn = k.inp("g_ffn", [L, D]); w_pq = k.inp("w_pq", [L, D, 2048])
    sk1 = k.inp("sub_k1", [L, 8, 128, 128]); sk2 = k.inp("sub_k2", [L, 8, 128, 128])
    pu = k.inp("peer_u", [L, 16384, D]); pv = k.inp("peer_v", [L, 16384, D])
    outn = k.outp("outn", [T, D])
    ztm = k.scratch("ztm", [T, 3072]); zfm = k.scratch("zfm", [32, 128, T]); zh = k.scratch("zh", [8, 128, 3])
    retT = k.scratch("retT", [8, 128, T]); rnnT = k.scratch("rnnT", [8, 128, T])
    x1 = k.scratch("x1", [T, D]); x2 = k.scratch("x2", [T, D])
    xc = [k.scratch(f"xcur{l}", [T, D]) for l in range(L)]
    kT = k.scratch("kT", [8, 128, 256]); vm = k.scratch("vm", [256, D])
    send_d = k.scratch("send_d", [128, 1024]); lsum_d = k.scratch("lsum_d", [128, 16])
    W = 1152
    xin = [k.scratch(f"xin{l}", [8 * 128, W]) for l in range(L)]
    xout = [k.scratch(f"xout{l}", [8 * 128, W]) for l in range(L)]
    hin = [k.scratch(f"hin{l}", [8 * 128, 128]) for l in range(L)]
    hout = [k.scratch(f"hout{l}", [8 * 128, 128]) for l in range(L)]
    k.consts()
    xl = x
    for l in range(L):
        k.stage_A(xl, xh if l == 0 else None, w_in[l], g_mix[l], pos, invf, ztm, zfm, zh)
        if l > 0:
            tails = zfm[0:8].rearrange("c p t -> p c t")[:, :, T - 3:T]
            k.stage_X([(zfm[cb][:, T - 3:T], cb * 3, 3) for cb in range(8)], oh, hin[l], hout[l], 128)
            k.stage_H(hout[l], ohp, zh, 128)
        k.stage_B1(ztm, send_d)
        k.stage_C(zfm, zh, pos, conv_w[l], conv_b[l], wa[l], ba[l], wx[l], bx[l], lam[l], "P1", lsum_out=lsum_d)
        k.stage_X([(send_d, 0, 1024), (lsum_d, 1024, 16)], oh, xin[l], xout[l], W)
        xo_v = xout[l].rearrange("(c p) w -> c p w", p=128)
        k.stage_B2(ztm, xo_v[:, :, 0:1024], csel, retT)
        k.stage_C(zfm, zh, pos, conv_w[l], conv_b[l], wa[l], ba[l], wx[l], bx[l], lam[l], "P2",
                  lsum_all=xo_v[:, :, 1024:1040], csel_d=csel, rnnT_d=rnnT)
        k.stage_D(retT, rnnT, zfm, w_ret[l], w_rnn[l], w_mix[l], xl, x1)
        k.stage_E0(mem, g_mem[l], w_xk[l], w_xv[l], kT, vm)
        k.stage_E(x1, x2, g_x[l], w_xq[l], w_xo[l], kT, vm)
        last = (l == L - 1)
        puf = (pu.rearrange("l e d -> (l e) d"), l * 16384 * D)
        pvf = (pv.rearrange("l e d -> (l e) d"), l * 16384 * D)
        k.stage_F(x2, xc[l], g_ffn[l], w_pq[l], sk1[l], sk2[l], puf, pvf, last, g_final if last else None,
                  outn if last else None)
        xl = xc[l]
    k.S.emit()
    return k


def run_fused(inputs, ncores=NCORES, T=SEQ // NCORES):
    x = np.ascontiguousarray(inputs["x"][0])
    pos = np.ascontiguousarray(inputs["positions"][0]).astype(np.int32)
    L = inputs["w_in"].shape[0]
    key = ("fused", T, L)
    if key not in _PROGS:
        _PROGS[key] = build_fused(T, L)
    k = _PROGS[key]
    names = ["w_in", "g_mix", "conv_w", "conv_b", "lru_wa", "lru_ba", "lru_wx", "lru_bx", "lru_lam", "w_ret_br",
             "w_rnn_br", "w_mix_out", "g_x", "g_mem", "w_xq", "w_xk", "w_xv", "w_xo", "g_ffn", "w_pq", "sub_k1",
             "sub_k2", "peer_u", "peer_v", "g_final"]
    common = {n: np.ascontiguousarray(inputs[n]) for n in names}
    common["mem"] = np.ascontiguousarray(inputs["mem"][0])
    common["invf"] = _invf()
    in_maps = []
    for c in range(ncores):
        cs = np.zeros(8, np.float32); cs[:c] = 1.0
        oh = np.zeros(8, np.float32); oh[c] = 1.0
        ohp = np.zeros(8, np.float32)
        if c > 0:
            ohp[c - 1] = 1.0
        xh = np.zeros((3, D), np.float32) if c == 0 else np.ascontiguousarray(x[c * T - 3:c * T])
        in_maps.append(dict(common, x=np.ascontiguousarray(x[c * T:(c + 1) * T]), xh=xh,
                            pos=np.ascontiguousarray(pos[c * T:(c + 1) * T]), csel=cs, oh=oh, ohp=ohp))
    r = run_bass_kernel_spmd(k.nc, in_maps, core_ids=list(range(ncores))).results
    return np.concatenate([r[c]["outn"] for c in range(ncores)], axis=0)


_INVF = None


def _invf():
    return (10000.0 ** (-(np.arange(0, 128, 2, dtype=np.float32) / np.float32(128)))).astype(np.float32)


_PROGS = {}


def _prog(name, T, final=False):
    key = (name, T, final)
    if key not in _PROGS:
        _PROGS[key] = build_P1(T) if name == "P1" else build_P2(T, final)
    return _PROGS[key]


def run_model(inputs, ncores=NCORES, T=SEQ // NCORES, dbg=False):
    x = np.ascontiguousarray(inputs["x"][0])
    pos = np.ascontiguousarray(inputs["positions"][0]).astype(np.int32)
    mem = np.ascontiguousarray(inputs["mem"][0])
    invf = _invf()
    L = inputs["w_in"].shape[0]
    cids = list(range(ncores))
    for l in range(L):
        xs = [np.ascontiguousarray(x[c * T:(c + 1) * T]) for c in range(ncores)]
        xhs = [np.zeros((3, D), np.float32) if c == 0 else np.ascontiguousarray(x[c * T - 3:c * T])
               for c in range(ncores)]
        poss = [np.ascontiguousarray(pos[c * T:(c + 1) * T]) for c in range(ncores)]
        common1 = dict(w_in=inputs["w_in"][l], g_mix=inputs["g_mix"][l], invf=invf, conv_w=inputs["conv_w"][l],
                       conv_b=inputs["conv_b"][l], lru_wa=inputs["lru_wa"][l], lru_ba=inputs["lru_ba"][l],
                       lru_wx=inputs["lru_wx"][l], lru_bx=inputs["lru_bx"][l], lru_lam=inputs["lru_lam"][l])
        common1 = {k_: np.ascontiguousarray(v) for k_, v in common1.items()}
        k1 = _prog("P1", T)
        in1 = [dict(common1, x=xs[c], xh=xhs[c], pos=poss[c]) for c in range(ncores)]
        r1 = run_bass_kernel_spmd(k1.nc, in1, core_ids=cids).results
        send_all = np.zeros((8, 128, 1024), np.float32)
        lsum_all = np.zeros((8, 128, 16), np.float32)
        for c in range(ncores):
            send_all[c] = r1[c]["send"]
            lsum_all[c] = r1[c]["lsum"]
        final = (l == L - 1)
        k2 = _prog("P2", T, True) if not dbg else build_P2(T, final, dbg=True, stages=dbg if isinstance(dbg, str) else "ABCDEF")
        common2 = dict(common1, send_all=send_all, lsum_all=lsum_all, w_ret_br=inputs["w_ret_br"][l],
                       w_rnn_br=inputs["w_rnn_br"][l], w_mix_out=inputs["w_mix_out"][l], mem=mem,
                       g_x=inputs["g_x"][l], g_mem=inputs["g_mem"][l], w_xq=inputs["w_xq"][l], w_xk=inputs["w_xk"][l],
                       w_xv=inputs["w_xv"][l], w_xo=inputs["w_xo"][l], g_ffn=inputs["g_ffn"][l], w_pq=inputs["w_pq"][l],
                       sub_k1=inputs["sub_k1"][l], sub_k2=inputs["sub_k2"][l], peer_u=inputs["peer_u"][l],
                       peer_v=inputs["peer_v"][l], g_final=inputs["g_final"])
        common2 = {k_: np.ascontiguousarray(v) for k_, v in common2.items()}
        in2 = []
        for c in range(ncores):
            cs = np.zeros(8, np.float32)
            cs[:c] = 1.0
            in2.append(dict(common2, x=xs[c], xh=xhs[c], pos=poss[c], csel=cs))
        r2 = run_bass_kernel_spmd(k2.nc, in2, core_ids=cids).results
        x = np.concatenate([r2[c]["out"] for c in range(ncores)], axis=0)
        if dbg:
            return r1, r2, x
    return np.concatenate([r2[c]["outn"] for c in range(ncores)], axis=0)


def kernel(**inputs):
    out = run_fused(inputs)
    return out[None].astype(np.float32)
```
